# Optimizing a Trainium2 kernel written in Bass

```python
import jax, jax.numpy as jnp
from jax import lax
import numpy as np

D_MODEL = 1024
BATCH = 2
SEQ = 16384
DEPTH = 1

HEAD_DIM = 64
ATTN_SCALE = HEAD_DIM ** -0.5
DIL_PAIRS = ((128, 1), (512, 4), (2048, 16))
N_DIL_GROUPS = len(DIL_PAIRS)
DIL_HEADS = 8
DIL_QBLK = 128
MOBA_HEADS = 8
MOBA_BLOCK = 256
MOBA_TOPK = 3
MOBA_QCHUNK = 64
MEM_TOKENS = 256
MEM_HEADS = 4
N_BRANCHES = 3
EPS = 1e-6
NEG_INF = -1e30

D_A = DIL_HEADS * HEAD_DIM
D_B = MOBA_HEADS * HEAD_DIM
D_M = MEM_HEADS * HEAD_DIM
IN_SIZES = [D_A] * (3 * N_DIL_GROUPS) + [D_A] + [D_B] * 4 + [D_M] * 2 + [N_BRANCHES * D_MODEL]
D_IN = int(sum(IN_SIZES))
IN_SPLIT_POINTS = tuple(int(v) for v in np.cumsum(IN_SIZES)[:-1])

kernel_name = "hybrid_dilated_moba_memory_gated_block"


def rmsnorm(x, w):
    xf = x.astype(jnp.float32)
    y = xf * lax.rsqrt(jnp.mean(xf * xf, axis=-1, keepdims=True) + EPS)
    return (y * w.astype(jnp.float32)).astype(x.dtype)


def alibi_slopes(n):
    return jnp.asarray(2.0 ** (-8.0 * np.arange(1, n + 1) / n), dtype=jnp.float32)


def dilated_window_group(q, k, v, slopes, window, dilation):
    B, S, H, Dh = q.shape
    n = S // dilation
    nb = n // DIL_QBLK
    span = window // dilation

    def to_blocks(t):
        t = t.reshape(B, n, dilation, H, Dh).transpose(0, 2, 3, 1, 4)
        return t.reshape(B, dilation, H, nb, DIL_QBLK, Dh)

    def band(t):
        prev = jnp.pad(t[:, :, :, :-1], ((0, 0), (0, 0), (0, 0), (1, 0), (0, 0), (0, 0)))
        return jnp.concatenate([prev, t], axis=4)

    qb = to_blocks(q)
    kb = band(to_blocks(k))
    vb = band(to_blocks(v))
    s = jnp.einsum("brhnqd,brhnkd->brhnqk", qb, kb, preferred_element_type=jnp.float32) * ATTN_SCALE
    a = jnp.arange(DIL_QBLK)[:, None]
    c = jnp.arange(2 * DIL_QBLK)[None, :]
    j = a + DIL_QBLK - c
    blk = jnp.arange(nb)[:, None, None]
    valid = (j >= 0) & (j <= span) & ((blk > 0) | (c >= DIL_QBLK))
    bias = -slopes[:, None, None] * (j * dilation).astype(jnp.float32)
    s = jnp.where(valid[None, None, None], s + bias[None, None, :, None], NEG_INF)
    m = jnp.max(s, axis=-1, keepdims=True)
    e = jnp.exp(s - m)
    den = jnp.sum(e, axis=-1)
    o = jnp.einsum("brhnqk,brhnkd->brhnqd", e.astype(v.dtype), vb,
                   preferred_element_type=jnp.float32) / den[..., None]
    lse = m[..., 0] + jnp.log(den)
    o = o.reshape(B, dilation, H, n, Dh).transpose(0, 3, 1, 2, 4).reshape(B, S, H, Dh)
    lse = lse.reshape(B, dilation, H, n).transpose(0, 3, 1, 2).reshape(B, S, H)
    return o, lse


def dilated_mixture(groups, slopes):
    B, S, H, Dh = groups[0][0].shape
    align = max(d for _, d in DIL_PAIRS) * DIL_QBLK
    S_pad = -(-S // align) * align

    def pad(t):
        return jnp.pad(t, ((0, 0), (0, S_pad - S), (0, 0), (0, 0)))

    outs, lses = [], []
    for (window, dilation), (q, k, v) in zip(DIL_PAIRS, groups):
        o, l = dilated_window_group(pad(q), pad(k), pad(v), slopes, window, dilation)
        outs.append(o[:, :S])
        lses.append(l[:, :S])
    w = jax.nn.softmax(jnp.stack(lses, 0), axis=0)
    o = jnp.einsum("gbsh,gbshd->bshd", w, jnp.stack(outs, 0))
    return o.astype(groups[0][0].dtype)


def moba_attention(q, k, v, slopes):
    B, S, H, Dh = q.shape
    S_pad = -(-S // MOBA_BLOCK) * MOBA_BLOCK
    padw = ((0, 0), (0, S_pad - S), (0, 0), (0, 0))
    q = jnp.pad(q, padw).transpose(0, 2, 1, 3)
    k = jnp.pad(k, padw).transpose(0, 2, 1, 3)
    v = jnp.pad(v, padw).transpose(0, 2, 1, 3)
    nblk = S_pad // MOBA_BLOCK
    kblk = k.reshape(B, H, nblk, MOBA_BLOCK, Dh)
    vblk = v.reshape(B, H, nblk, MOBA_BLOCK, Dh)
    kmean = jnp.mean(kblk.astype(jnp.float32), axis=3)
    gate = jnp.einsum("bhsd,bhnd->bhsn", q.astype(jnp.float32), kmean)
    qblk_id = jnp.arange(S_pad) // MOBA_BLOCK
    past = jnp.arange(nblk)[None, :] < qblk_id[:, None]
    gate = jnp.where(past[None, None], gate, NEG_INF)
    topk = min(MOBA_TOPK, nblk)
    _, sel = lax.top_k(gate, topk)
    sel_valid = jnp.arange(topk)[None, :] < qblk_id[:, None]
    bi = jnp.arange(B)[:, None, None, None]
    hi = jnp.arange(H)[None, :, None, None]
    koff = jnp.arange(MOBA_BLOCK)

    def chunk(ci):
        start = ci * MOBA_QCHUNK
        qc = lax.dynamic_slice_in_dim(q, start, MOBA_QCHUNK, axis=2)
        selc = lax.dynamic_slice_in_dim(sel, start, MOBA_QCHUNK, axis=2)
        validc = lax.dynamic_slice_in_dim(sel_valid, start, MOBA_QCHUNK, axis=0)
        k_sel = kblk[bi, hi, selc]
        v_sel = vblk[bi, hi, selc]
        own = start // MOBA_BLOCK
        k_own = lax.dynamic_index_in_dim(kblk, own, axis=2, keepdims=False)
        v_own = lax.dynamic_index_in_dim(vblk, own, axis=2, keepdims=False)
        t = start + jnp.arange(MOBA_QCHUNK)
        s_sel = jnp.einsum("bhqd,bhqnkd->bhqnk", qc, k_sel,
                           preferred_element_type=jnp.float32) * ATTN_SCALE
        pos_sel = selc[..., None] * MOBA_BLOCK + koff
        dist_sel = (t[None, None, :, None, None] - pos_sel).astype(jnp.float32)
        s_sel = s_sel - slopes[None, :, None, None, None] * dist_sel
        s_sel = jnp.where(validc[None, None, :, :, None], s_sel, NEG_INF)
        s_own = jnp.einsum("bhqd,bhkd->bhqk", qc, k_own,
                           preferred_element_type=jnp.float32) * ATTN_SCALE
        dist_own = t[:, None] - (own * MOBA_BLOCK + koff)[None, :]
        s_own = jnp.where(dist_own[None, None] >= 0,
                          s_own - slopes[None, :, None, None] * dist_own.astype(jnp.float32)[None, None],
                          NEG_INF)
        s_all = jnp.concatenate([s_sel.reshape(B, H, MOBA_QCHUNK, topk * MOBA_BLOCK), s_own], axis=-1)
        p = jax.nn.softmax(s_all, axis=-1).astype(v.dtype)
        p_sel = p[..., : topk * MOBA_BLOCK].reshape(B, H, MOBA_QCHUNK, topk, MOBA_BLOCK)
        p_own = p[..., topk * MOBA_BLOCK:]
        o = jnp.einsum("bhqnk,bhqnkd->bhqd", p_sel, v_sel, preferred_element_type=jnp.float32)
        o = o + jnp.einsum("bhqk,bhkd->bhqd", p_own, v_own, preferred_element_type=jnp.float32)
        return o

    n_chunks = S_pad // MOBA_QCHUNK
    o = lax.map(chunk, jnp.arange(n_chunks))
    o = o.transpose(1, 2, 0, 3, 4).reshape(B, H, S_pad, Dh).transpose(0, 2, 1, 3)[:, :S]
    return o.astype(q.dtype)


def memory_cross_attention(q, k, v):
    s = jnp.einsum("bshd,bmhd->bhsm", q, k, preferred_element_type=jnp.float32) * ATTN_SCALE
    p = jax.nn.softmax(s, axis=-1).astype(v.dtype)
    return jnp.einsum("bhsm,bmhd->bshd", p, v)


def setup_inputs(seed: int = 0) -> dict:
    key = jax.random.key(seed)
    ks = jax.random.split(key, 12)
    f32 = jnp.float32
    x = jax.random.normal(ks[0], (BATCH, SEQ, D_MODEL), f32)
    mem = jax.random.normal(ks[1], (BATCH, MEM_TOKENS, D_MODEL), f32)
    norm_w = 1.0 + 0.02 * jax.random.normal(ks[2], (DEPTH, D_MODEL), f32)
    mem_norm_w = 1.0 + 0.02 * jax.random.normal(ks[3], (DEPTH, D_MODEL), f32)
    w_in = jax.random.normal(ks[4], (DEPTH, D_MODEL, D_IN), f32) * D_MODEL ** -0.5
    b_merge = 0.1 * jax.random.normal(ks[5], (DEPTH, N_BRANCHES * D_MODEL), f32)
    w_mem_kv = jax.random.normal(ks[6], (DEPTH, D_MODEL, 2 * D_M), f32) * D_MODEL ** -0.5
    w_branch_a = jax.random.normal(ks[7], (DEPTH, D_A, D_MODEL), f32) * D_A ** -0.5
    w_branch_b = jax.random.normal(ks[8], (DEPTH, D_B, D_MODEL), f32) * D_B ** -0.5
    w_branch_m = jax.random.normal(ks[9], (DEPTH, D_M, D_MODEL), f32) * D_M ** -0.5
    w_out = jax.random.normal(ks[10], (DEPTH, D_MODEL, D_MODEL), f32) * D_MODEL ** -0.5
    final_norm_w = 1.0 + 0.02 * jax.random.normal(ks[11], (D_MODEL,), f32)
    return {"x": x, "mem": mem, "norm_w": norm_w, "mem_norm_w": mem_norm_w, "w_in": w_in,
            "b_merge": b_merge, "w_mem_kv": w_mem_kv, "w_branch_a": w_branch_a,
            "w_branch_b": w_branch_b, "w_branch_m": w_branch_m, "w_out": w_out,
            "final_norm_w": final_norm_w}


def reference(x, mem, norm_w, mem_norm_w, w_in, b_merge, w_mem_kv, w_branch_a, w_branch_b,
              w_branch_m, w_out, final_norm_w):
    B, S, _ = x.shape
    slopes = alibi_slopes(DIL_HEADS + MOBA_HEADS)
    slopes_a = slopes[0::2]
    slopes_b = slopes[1::2]

    def heads(t):
        return t.reshape(t.shape[0], t.shape[1], -1, HEAD_DIM)

    for l in range(DEPTH):
        h = rmsnorm(x, norm_w[l])
        proj = h @ w_in[l]
        pieces = jnp.split(proj, IN_SPLIT_POINTS, axis=-1)
        a_qkv = pieces[: 3 * N_DIL_GROUPS]
        a_gate = pieces[3 * N_DIL_GROUPS]
        b_q, b_k, b_v, b_gate = pieces[3 * N_DIL_GROUPS + 1: 3 * N_DIL_GROUPS + 5]
        m_q, m_gate = pieces[3 * N_DIL_GROUPS + 5: 3 * N_DIL_GROUPS + 7]
        merge_logits = pieces[3 * N_DIL_GROUPS + 7]

        groups = [(heads(a_qkv[3 * g]), heads(a_qkv[3 * g + 1]), heads(a_qkv[3 * g + 2]))
                  for g in range(N_DIL_GROUPS)]
        o_a = dilated_mixture(groups, slopes_a).reshape(B, S, D_A)
        o_b = moba_attention(heads(b_q), heads(b_k), heads(b_v), slopes_b).reshape(B, S, D_B)
        kv = rmsnorm(mem, mem_norm_w[l]) @ w_mem_kv[l]
        m_k, m_v = jnp.split(kv, 2, axis=-1)
        o_m = memory_cross_attention(heads(m_q), heads(m_k), heads(m_v)).reshape(B, S, D_M)

        p_a = (o_a * jax.nn.silu(a_gate)) @ w_branch_a[l]
        p_b = (o_b * jax.nn.silu(b_gate)) @ w_branch_b[l]
        p_m = (o_m * jax.nn.silu(m_gate)) @ w_branch_m[l]
        g_a, g_b, g_m = jnp.split(jax.nn.sigmoid(merge_logits + b_merge[l]), N_BRANCHES, axis=-1)
        merged = g_a * p_a + g_b * p_b + g_m * p_m
        x = x + merged @ w_out[l]
    return rmsnorm(x, final_norm_w)
```

```python
import contextlib
import numpy as np
import ml_dtypes
import concourse.bass as bass
import concourse.mybir as mybir
from concourse.bass_utils import run_bass_kernel_spmd

F32 = mybir.dt.float32
BF16 = mybir.dt.bfloat16
AF = mybir.ActivationFunctionType
ALU = mybir.AluOpType
AX = mybir.AxisListType

D = 1024
T = 4096
VT = 16384
HALO = 2048
HT = HALO + T
DIN = 10752
DILS = (1, 4, 16)
SCALE = 0.125
EPS = 1e-6
NEG = -1e30
MBNEG = -30000.0
NM = 160
SKIP_FAR = True
SKIP_NATS = 164.0


class Tr:
    EPOCH = 16000
    DEPOCH = 1000

    def __init__(self, nc):
        self.nc = nc
        self.eng = {"pe": nc.tensor, "act": nc.scalar, "dve": nc.vector, "pool": nc.gpsimd, "sp": nc.sync}
        self.cnt = {e: 0 for e in ("pe", "act", "dve", "pool")}
        self.esem = {e: [] for e in self.cnt}
        self.slot = {}
        self.lastw = {}
        self.readers = {}
        self.waited = {e: {} for e in self.eng}
        self.nsem = 0
        self.nwait = 0
        self.ndma = 0
        self.sems = {}
        self.free_slots = []

    def _newsem(self, name):
        s = self.nc.alloc_semaphore(name)
        self.nsem += 1
        self.sems[id(s)] = s
        return s

    @staticmethod
    def _add(deps, ev):
        k = id(ev[0])
        if k not in deps or deps[k][1] < ev[1]:
            deps[k] = ev

    def _deps(self, reads, writes):
        deps = {}
        for r in reads:
            if r in self.lastw:
                self._add(deps, self.lastw[r])
        for w in writes:
            if w in self.lastw:
                self._add(deps, self.lastw[w])
            for ev in self.readers.get(w, {}).values():
                self._add(deps, ev)
        return deps

    def _emit_waits(self, eng, deps):
        e = self.eng[eng]
        wd = self.waited[eng]
        for k, (sem, val, peng) in deps.items():
            if peng == eng and eng == "pe":
                continue
            if wd.get(k, 0) >= val:
                continue
            e.wait_ge(sem, val)
            self.nwait += 1
            wd[k] = val

    def _record(self, ev, reads, writes):
        for w in writes:
            self.lastw[w] = ev
            self.readers[w] = {}
        for r in reads:
            if r in writes:
                continue
            self._add(self.readers.setdefault(r, {}), ev)

    def op(self, eng, fn, reads=(), writes=()):
        deps = self._deps(reads, writes)
        self._emit_waits(eng, deps)
        ins = fn()
        n = self.cnt[eng]
        ep = n // self.EPOCH
        if ep >= len(self.esem[eng]):
            self.esem[eng].append(self._newsem("e_%s_%d" % (eng, ep)))
        sem = self.esem[eng][ep]
        val = n % self.EPOCH + 1
        ins.then_inc(sem, 1)
        self.cnt[eng] = n + 1
        ev = (sem, val, eng)
        self._record(ev, reads, writes)
        return ev

    def dma(self, q, out, in_, reads=(), writes=(), slot=None):
        deps = self._deps(reads, writes)
        st = self.slot.get(slot)
        if st is None and self.free_slots:
            st = self.free_slots.pop()
            self.slot[slot] = st
        if st is None or st[1] >= self.DEPOCH:
            if st is not None:
                self._add(deps, (st[0], 16 * st[1], "dma"))
            st = [self._newsem("d_%s" % slot), 0]
            self.slot[slot] = st
        elif st[1] > 0:
            self._add(deps, (st[0], 16 * st[1], "dma"))
        self._emit_waits(q, deps)
        ins = self.eng[q].dma_start(out=out, in_=in_)
        st[1] += 1
        self.ndma += 1
        ins.then_inc(st[0], 16)
        ev = (st[0], 16 * st[1], "dma")
        self._record(ev, reads, writes)
        return ev

    def _all_events(self):
        deps = {}
        for e, n in self.cnt.items():
            if n > 0:
                ep = (n - 1) // self.EPOCH
                self._add(deps, (self.esem[e][ep], (n - 1) % self.EPOCH + 1, "x"))
        for st in list(self.slot.values()) + self.free_slots:
            if st[1] > 0:
                self._add(deps, (st[0], 16 * st[1], "dma"))
        return deps

    def barrier(self):
        deps = self._all_events()
        for e in self.eng:
            self._emit_waits(e, dict(deps))
        self.lastw = {}
        self.readers = {}
        self.free_slots.extend(self.slot.values())
        self.slot = {}

    def finish(self):
        self._emit_waits("sp", self._all_events())


class Rot:
    def __init__(self, tiles, name):
        self.tiles = tiles
        self.name = name
        self.i = 0

    def next(self):
        k = self.i % len(self.tiles)
        self.i += 1
        return self.tiles[k], "%s%d" % (self.name, k)


def build_program(dbg=()):
    nc = bass.Bass("TRN2", target_bir_lowering=False)
    tr = Tr(nc)

    def din(name, shape, dt=F32):
        return nc.dram_tensor(name, list(shape), dt, kind="ExternalInput").ap()

    def dscr(name, shape, dt):
        kind = "ExternalOutput" if name in dbg else "Internal"
        return nc.dram_tensor(name, list(shape), dt, kind=kind).ap()

    xs = din("xs", [VT, D])
    memb = din("memb", [256, D])
    w_in = din("w_in", [D, DIN])
    w_kv = din("w_mem_kv", [D, 512])
    w_ba = din("w_branch_a", [512, D])
    w_bb = din("w_branch_b", [512, D])
    w_bm = din("w_branch_m", [256, D])
    w_o = din("w_out", [D, D])
    nw_d = din("nw", [128, 8])
    mnw_d = din("mnw", [128, 8])
    bmg_d = din("bmg", [128, 24])
    fnw_d = din("fnw", [128, D])
    ident_d = din("ident", [128, 128], BF16)
    identf_d = din("identf", [128, 128])
    bm_d = din("bm", [128, 3 * 8 * 256])
    sel_d = din("sel", [64, VT], BF16)
    ab_d = din("ab", [128, 8 * NM])
    ct_d = din("ct", [128, 32])
    pm_d = din("pm", [128, 128])
    cb_d = din("cb", [128, 4 * 512])
    vb_d = din("vb", [128, 64])
    hv_d = din("hv", [128, 1])
    out_d = nc.dram_tensor("out", [T, D], F32, kind="ExternalOutput").ap()

    KTb = dscr("KTb", [512, VT], BF16)
    Vb = dscr("Vb", [VT, 512], BF16)
    QTb = dscr("QTb", [512, T], BF16)
    QTg = [dscr("QTg%d" % g, [512, T], BF16) for g in range(3)]
    KTg = [dscr("KTg%d" % g, [512, HT], BF16) for g in range(3)]
    Vg = [dscr("Vg%d" % g, [HT, 512], BF16) for g in range(3)]
    Ga = dscr("Ga", [T, 512], BF16)
    Gb = dscr("Gb", [T, 512], BF16)
    Gm = dscr("Gm", [T, 256], BF16)
    MQT = dscr("MQT", [256, T], BF16)
    SG = dscr("SG", [3072, T], BF16)
    OD = [dscr("OD%d" % g, [T, 8, 65], F32) for g in range(3)]
    OB = dscr("OB", [T, 8, 65], F32)
    OM = dscr("OM", [T, 256], F32)

    es = contextlib.ExitStack()

    def sb(stack, name, shape, dt):
        return stack.enter_context(nc.sbuf_tensor("s_" + name, list(shape), dt))

    with es:
        ident = sb(es, "ident", [128, 128], BF16)
        identf = sb(es, "identf", [128, 128], F32)
        nw = sb(es, "nw", [128, 8], F32)
        mnw = sb(es, "mnw", [128, 8], F32)
        hv = sb(es, "hv", [128, 1], F32)
        PS = [es.enter_context(nc.psum_tensor("ps%d" % i, [128, 512], F32)) for i in range(6)]
        PB = [es.enter_context(nc.psum_tensor("pb%d" % i, [128, 1024], BF16)) for i in range(2)]
        for i, (dst, src) in enumerate([(ident, ident_d), (identf, identf_d), (nw, nw_d), (mnw, mnw_d), (hv, hv_d)]):
            tr.dma("sp", dst[:], src, writes=["c%d" % i], slot="ld%d" % (i % 2))
        tr.barrier()

        evac_i = [0]

        def evac(out, in_, rd, wr, func=None, bias=None, eng=None):
            if func is not None:
                e = "act"
            elif eng is not None:
                e = eng
            else:
                e = "act" if evac_i[0] % 2 == 0 else "dve"
                evac_i[0] += 1
            if e == "act":
                if func is None:
                    tr.op("act", lambda: nc.scalar.copy(out=out, in_=in_), reads=rd, writes=wr)
                elif bias is None:
                    tr.op("act", lambda: nc.scalar.activation(out=out, in_=in_, func=func), reads=rd, writes=wr)
                else:
                    tr.op("act", lambda: nc.scalar.activation(out=out, in_=in_, func=func, bias=bias), reads=rd, writes=wr)
            else:
                tr.op("dve", lambda: nc.vector.tensor_copy(out=out, in_=in_), reads=rd, writes=wr)

        cv_i = [0]

        def conv_scale(out, in_, sc, rd, wr):
            e = ("dve", "pool", "act")[cv_i[0] % 3]
            cv_i[0] += 1
            if e == "act":
                if sc is None:
                    tr.op("act", lambda: nc.scalar.copy(out=out, in_=in_), reads=rd, writes=wr)
                else:
                    tr.op("act", lambda: nc.scalar.activation(out=out, in_=in_, func=AF.Copy, scale=sc), reads=rd, writes=wr)
            else:
                en = nc.vector if e == "dve" else nc.gpsimd
                if sc is None:
                    tr.op(e, lambda: en.tensor_copy(out=out, in_=in_), reads=rd, writes=wr)
                else:
                    tr.op(e, lambda: en.tensor_scalar(out=out, in0=in_, scalar1=sc, scalar2=None, op0=ALU.mult),
                          reads=rd, writes=wr)

        def rms_tile(st, xt, xres, nsub, hb, hbres, ssb, ssres):
            tr.op("dve", lambda: nc.vector.memset(ssb[:], 0.0), writes=[ssres])
            junk = st["junk"]
            for s in range(nsub):
                tr.op("act", lambda s=s: nc.scalar.activation(out=junk[:], in_=xt[:, s, :], func=AF.Square,
                                                               accum_out=ssb[:, s:s + 1]),
                      reads=[xres, ssres], writes=["junk", ssres])
            tr.op("act", lambda: nc.scalar.activation(out=ssb[:, nsub:2 * nsub], in_=ssb[:, 0:nsub], func=AF.Sqrt,
                                                       bias=EPS, scale=1.0 / D), reads=[ssres], writes=[ssres])
            tr.op("dve", lambda: nc.vector.reciprocal(out=ssb[:, 2 * nsub:3 * nsub], in_=ssb[:, nsub:2 * nsub]),
                  reads=[ssres], writes=[ssres])
            for s in range(nsub):
                sc = ssb[:, 2 * nsub + s:2 * nsub + s + 1]
                if s % 2 == 0:
                    tr.op("dve", lambda s=s, sc=sc: nc.vector.tensor_scalar(out=hb[:, s, :], in0=xt[:, s, :], scalar1=sc,
                                                                           scalar2=None, op0=ALU.mult),
                          reads=[xres, ssres], writes=[hbres])
                else:
                    tr.op("act", lambda s=s, sc=sc: nc.scalar.activation(out=hb[:, s, :], in_=xt[:, s, :], func=AF.Copy, scale=sc),
                          reads=[xres, ssres], writes=[hbres])

        with contextlib.ExitStack() as p1:
            hT = sb(p1, "hT", [128, 8, HT], BF16)
            wst = sb(p1, "wst", [128, 8, 512], F32)
            wbf = [sb(p1, "wbf%d" % i, [128, 8, 512], BF16) for i in range(2)]
            junk = sb(p1, "junk", [128, D], BF16)
            st = {"junk": junk}
            bmg = sb(p1, "bmg", [128, 24], F32)
            tr.dma("sp", bmg[:], bmg_d, writes=["bmg"], slot="ld0")

            def load_w(col0, ncols, dst, dres, src=w_in, scale=nw):
                tr.dma("sp", wst[:, :, 0:ncols], src[:, col0:col0 + ncols].rearrange("(fc p) c -> p fc c", p=128),
                       writes=["wst"], slot="wst")
                for fc in range(8):
                    conv_scale(dst[:, fc, 0:ncols], wst[:, fc, 0:ncols], scale[:, fc:fc + 1], ["wst"], [dres])

            with contextlib.ExitStack() as p1a:
                xts = Rot([sb(p1a, "xt%d" % i, [128, 2, D], F32) for i in range(4)], "xt")
                hbs = Rot([sb(p1a, "hb%d" % i, [128, 2, D], BF16) for i in range(2)], "hb")
                sss = Rot([sb(p1a, "ss%d" % i, [128, 6], F32) for i in range(2)], "ss")
                hts = Rot([sb(p1a, "htt%d" % i, [128, 8, 256], BF16) for i in range(2)], "htt")
                ksts = Rot([sb(p1a, "kst%d" % i, [128, 4, 256], BF16) for i in range(2)], "kst")
                vsts = Rot([sb(p1a, "vst%d" % i, [128, 2, 512], BF16) for i in range(2)], "vst")
                load_w(5632, 512, wbf[0], "wbf0")
                load_w(6144, 512, wbf[1], "wbf1")
                NT1 = VT // 256
                pbi = 0
                psi = 0
                nst = {}

                xld = {}

                def stage_l(i):
                    xt, xres = xts.next()
                    tr.dma("sp", xt[:], xs[256 * i:256 * i + 256, :].rearrange("(s p) f -> p s f", p=128),
                           writes=[xres], slot=xres)
                    xld[i] = (xt, xres)

                def stage_n(i):
                    if i + 2 < NT1:
                        stage_l(i + 2)
                    xt, xres = xld.pop(i)
                    hb, hbres = hbs.next()
                    ssb, ssres = sss.next()
                    rms_tile(st, xt, xres, 2, hb, hbres, ssb, ssres)
                    nst[i] = (hb, hbres)

                mst = {}
                cnt1 = {"pbi": 0, "psi": 0}

                def stage_mt(i):
                    hb, hbres = nst.pop(i)
                    resident = (256 * i >= VT - HT)
                    if resident:
                        u0 = 256 * i - (VT - HT)
                        hdst = lambda fc, u0=u0: hT[:, fc, u0:u0 + 256]
                        hres = "hTres%d" % i
                    else:
                        htt, hres = hts.next()
                        hdst = lambda fc, htt=htt: htt[:, fc, :]
                    mst[i] = (hdst, hres)
                    for fc in range(8):
                        pb = PB[cnt1["pbi"] % 2]
                        pbres = "pb%d" % (cnt1["pbi"] % 2)
                        cnt1["pbi"] += 1
                        for s in range(2):
                            tr.op("pe", lambda s=s, fc=fc, pb=pb: nc.tensor.transpose(
                                out=pb[:, s * 128:(s + 1) * 128], in_=hb[:, s, fc * 128:(fc + 1) * 128], identity=ident[:]),
                                reads=[hbres], writes=[pbres])
                        evac(hdst(fc), pb[:, 0:256], [pbres], [hres])

                def stage_mm(i):
                    hdst, hres = mst.pop(i)
                    kst, kres = ksts.next()
                    for pr in range(4):
                        ps = PS[cnt1["psi"] % 6]
                        psres = "ps%d" % (cnt1["psi"] % 6)
                        cnt1["psi"] += 1
                        for fc in range(8):
                            tr.op("pe", lambda fc=fc, pr=pr, ps=ps: nc.tensor.matmul(
                                ps[:, 0:256], lhsT=wbf[0][:, fc, pr * 128:(pr + 1) * 128], rhs=hdst(fc),
                                start=(fc == 0), stop=(fc == 7)), reads=["wbf0", hres], writes=[psres])
                        evac(kst[:, pr, :], ps[:, 0:256], [psres], [kres])
                    tr.dma("pool", KTb[:, 256 * i:256 * i + 256].rearrange("(pr p) t -> p pr t", p=128), kst[:],
                           reads=[kres], writes=[], slot=kres)
                    vst, vres = vsts.next()
                    for s in range(2):
                        ps = PS[cnt1["psi"] % 6]
                        psres = "ps%d" % (cnt1["psi"] % 6)
                        cnt1["psi"] += 1
                        for fc in range(8):
                            tr.op("pe", lambda fc=fc, s=s, ps=ps: nc.tensor.matmul(
                                ps[:, :], lhsT=hdst(fc)[:, s * 128:(s + 1) * 128], rhs=wbf[1][:, fc, :],
                                start=(fc == 0), stop=(fc == 7)), reads=["wbf1", hres], writes=[psres])
                        evac(vst[:, s, :], ps[:, :], [psres], [vres])
                    tr.dma("pool", Vb[256 * i:256 * i + 256, :].rearrange("(s p) c -> p s c", p=128), vst[:],
                           reads=[vres], writes=[], slot=vres)

                stage_l(0)
                stage_l(1)
                stage_n(0)
                stage_n(1)
                stage_mt(0)
                for i in range(NT1):
                    if i + 1 < NT1:
                        stage_mt(i + 1)
                    if i + 2 < NT1:
                        stage_n(i + 2)
                    stage_mm(i)
                tr.barrier()

            with contextlib.ExitStack() as p1b:
                fsts = Rot([sb(p1b, "fst%d" % i, [128, 512], BF16) for i in range(4)], "fst")
                gths = Rot([sb(p1b, "gth%d" % i, [128, 8, 512], BF16) for i in range(3)], "gth")
                gst = [None]
                psi = [0]

                blocks = []

                def fm_block(col0, ncols, tiles, func=None, bias_col0=None, gather=False):
                    def units(wb, wres):
                        us = []
                        if gather:
                            for (tsl, N, dstf) in tiles:
                                def ug(tsl=tsl, N=N):
                                    gt_, gres = gths.next()
                                    for fc in range(8):
                                        conv_scale(gt_[:, fc, 0:N], hT[:, fc, tsl], None, [], [gres])
                                    gst[0] = (gt_, gres)
                                us.append(ug)
                                for cc in range(ncols // 128):
                                    def u(cc=cc, N=N, dstf=dstf):
                                        gt_, gres = gst[0]
                                        ps = PS[psi[0] % 6]
                                        psres = "ps%d" % (psi[0] % 6)
                                        psi[0] += 1
                                        for fc in range(8):
                                            tr.op("pe", lambda fc=fc: nc.tensor.matmul(
                                                ps[:, 0:N], lhsT=wb[:, fc, cc * 128:(cc + 1) * 128], rhs=gt_[:, fc, 0:N],
                                                start=(fc == 0), stop=(fc == 7)), reads=[wres, gres], writes=[psres])
                                        fst, fres = fsts.next()
                                        evac(fst[:, 0:N], ps[:, 0:N], [psres], [fres], func=func)
                                        tr.dma("pool" if psi[0] % 2 else "sp", dstf(cc), fst[:, 0:N], reads=[fres], writes=[], slot=fres)
                                    us.append(u)
                            return us
                        for cc in range(ncols // 128):
                            for (tsl, N, dstf) in tiles:
                                def u(cc=cc, tsl=tsl, N=N, dstf=dstf):
                                    ps = PS[psi[0] % 6]
                                    psres = "ps%d" % (psi[0] % 6)
                                    psi[0] += 1
                                    for fc in range(8):
                                        tr.op("pe", lambda fc=fc: nc.tensor.matmul(
                                            ps[:, 0:N], lhsT=wb[:, fc, cc * 128:(cc + 1) * 128], rhs=hT[:, fc, tsl],
                                            start=(fc == 0), stop=(fc == 7)), reads=[wres], writes=[psres])
                                    fst, fres = fsts.next()
                                    b = None if bias_col0 is None else bmg[:, bias_col0 + cc:bias_col0 + cc + 1]
                                    evac(fst[:, 0:N], ps[:, 0:N], [psres], [fres], func=func, bias=b)
                                    tr.dma("pool" if psi[0] % 2 else "sp", dstf(cc), fst[:, 0:N], reads=[fres], writes=[], slot=fres)
                                us.append(u)
                        return us
                    blocks.append((col0, ncols, units))

                def tm_block(col0, ncols, tiles, func=None, gather=False):
                    def units(wb, wres):
                        us = []
                        for (tsl, dst) in tiles:
                            def u(tsl=tsl, dst=dst):
                                if gather:
                                    gt_, gres = gths.next()
                                    for fc in range(8):
                                        conv_scale(gt_[:, fc, 0:128], hT[:, fc, tsl], None, [], [gres])
                                    lt = lambda fc: gt_[:, fc, 0:128]
                                    rds = [wres, gres]
                                else:
                                    lt = lambda fc: hT[:, fc, tsl]
                                    rds = [wres]
                                ps = PS[psi[0] % 6]
                                psres = "ps%d" % (psi[0] % 6)
                                psi[0] += 1
                                for fc in range(8):
                                    tr.op("pe", lambda fc=fc: nc.tensor.matmul(
                                        ps[:, 0:ncols], lhsT=lt(fc), rhs=wb[:, fc, 0:ncols],
                                        start=(fc == 0), stop=(fc == 7)), reads=rds, writes=[psres])
                                fst, fres = fsts.next()
                                evac(fst[:, 0:ncols], ps[:, 0:ncols], [psres], [fres], func=func)
                                tr.dma("pool" if psi[0] % 2 else "sp", dst, fst[:, 0:ncols], reads=[fres], writes=[], slot=fres)
                            us.append(u)
                        return us
                    blocks.append((col0, ncols, units))

                own_fm = lambda dst: [(slice(HALO + 512 * i, HALO + 512 * i + 512), 512,
                                       (lambda cc, i=i: dst[cc * 128:(cc + 1) * 128, 512 * i:512 * i + 512]))
                                      for i in range(8)]
                own_tm = lambda dst, nco: [(slice(HALO + 128 * i, HALO + 128 * i + 128), dst[128 * i:128 * i + 128, 0:nco])
                                           for i in range(32)]
                for g, d in enumerate(DILS):
                    c0 = 1536 * g
                    tiles = []
                    nq = T // d
                    for r in range(d):
                        N = min(512, nq)
                        for i0 in range(0, nq, N):
                            u0 = HALO + r + d * i0
                            tiles.append((slice(u0, u0 + d * (N - 1) + 1, d), N,
                                          (lambda cc, r=r, i0=i0, N=N, g=g, nq=nq:
                                           QTg[g][cc * 128:(cc + 1) * 128, r * nq + i0:r * nq + i0 + N])))
                    fm_block(c0, 512, tiles, gather=(d > 1))
                    tiles = []
                    nk = HT // d
                    i_first = HALO // d - 128
                    for r in range(d):
                        i0 = i_first
                        while i0 < nk:
                            N = min(512, nk - i0)
                            u0 = r + d * i0
                            tiles.append((slice(u0, u0 + d * (N - 1) + 1, d), N,
                                          (lambda cc, r=r, i0=i0, N=N, g=g, nk=nk:
                                           KTg[g][cc * 128:(cc + 1) * 128, r * nk + i0:r * nk + i0 + N])))
                            i0 += N
                    fm_block(c0 + 512, 512, tiles, gather=(d > 1))
                    tiles = []
                    for r in range(d):
                        for kb in range(HALO // d // 128 - 1, nk // 128):
                            u0 = r + d * 128 * kb
                            p0 = r * nk + 128 * kb
                            tiles.append((slice(u0, u0 + d * 127 + 1, d), Vg[g][p0:p0 + 128, :]))
                    tm_block(c0 + 1024, 512, tiles, gather=(d > 1))
                tm_block(4608, 512, own_tm(Ga, 512), func=AF.Silu)
                fm_block(5120, 512, own_fm(QTb))
                tm_block(6656, 512, own_tm(Gb, 512), func=AF.Silu)
                fm_block(7168, 256, own_fm(MQT))
                tm_block(7424, 256, own_tm(Gm, 256), func=AF.Silu)
                for j in range(6):
                    fm_block(7680 + 512 * j, 512,
                             [(sl, N, (lambda cc, f=f, j=j: f(cc + 4 * j))) for (sl, N, f) in own_fm(SG)],
                             func=AF.Sigmoid, bias_col0=4 * j)

                def w_dma(n):
                    col0, ncols, _ = blocks[n]
                    tr.dma("sp", wst[:, :, 0:ncols], w_in[:, col0:col0 + ncols].rearrange("(fc p) c -> p fc c", p=128),
                           writes=["wst"], slot="wst")

                def w_conv(n):
                    col0, ncols, _ = blocks[n]
                    k = n % 2
                    for fc in range(8):
                        conv_scale(wbf[k][:, fc, 0:ncols], wst[:, fc, 0:ncols], nw[:, fc:fc + 1], ["wst"], ["wbf%d" % k])

                w_dma(0)
                w_conv(0)
                for n in range(len(blocks)):
                    if n + 1 < len(blocks):
                        w_dma(n + 1)
                    us = blocks[n][2](wbf[n % 2], "wbf%d" % (n % 2))
                    for ui, u in enumerate(us):
                        if ui == (2 * len(us)) // 3 and n + 1 < len(blocks):
                            w_conv(n + 1)
                        u()
                tr.barrier()

        if "stop1" in dbg:
            tr.finish()
            return nc

        with contextlib.ExitStack() as p2:
            bm = sb(p2, "bm", [128, 3 * 8 * 256], F32)
            tr.dma("sp", bm[:], bm_d, writes=["bm"], slot="ld0")
            qts = Rot([sb(p2, "qt%d" % i, [128, T], BF16) for i in range(2)], "qt")
            kts = Rot([sb(p2, "kt%d" % i, [128, HT], BF16) for i in range(2)], "kt")
            vas = Rot([sb(p2, "va%d" % i, [128, 48, 2, 65], BF16) for i in range(2)], "va")
            efs = Rot([sb(p2, "ef%d" % i, [128, 256], F32) for i in range(3)], "ef")
            ems = Rot([sb(p2, "em%d" % i, [128, 256], BF16) for i in range(4)], "em")
            osbs = Rot([sb(p2, "osb%d" % i, [128, 32, 2, 65], F32) for i in range(2)], "osb")
            for va in vas.tiles:
                tr.op("pool", lambda va=va: nc.gpsimd.memset(va[:, :, :, 64:65], 1.0), writes=["va0", "va1"])
            tr.barrier()
            for g, d in enumerate(DILS):
                nk = HT // d
                nq = T // d
                nkb = nk // 128
                qb0 = nkb - nq // 128
                nqo = nq // 128
                for hp in range(4):
                    qt, qres = qts.next()
                    kt, kres = kts.next()
                    va, vres = vas.next()
                    tr.dma("sp", qt[:], QTg[g][hp * 128:(hp + 1) * 128, :], writes=[qres], slot=qres)
                    tr.dma("sp", kt[:], KTg[g][hp * 128:(hp + 1) * 128, :], writes=[kres], slot=kres)
                    for hh in range(2):
                        for part in range(2):
                            tr.dma("sp", va[:, 24 * part:24 * part + 24, hh, 0:64],
                                   Vg[g][3072 * part:3072 * part + 3072, hp * 128 + 64 * hh:hp * 128 + 64 * hh + 64].rearrange(
                                       "(c p) e -> p c e", p=128),
                                   writes=[vres], slot="%sp%d" % (vres, part % 2))
                    steps = []
                    for r in range(d):
                        osb, ores = osbs.next()
                        for hh in range(2):
                            for kb in range(qb0 - 1, nkb):
                                has_cur = kb >= qb0
                                has_prev = kb + 1 < nkb
                                if not (has_cur or has_prev):
                                    continue
                                steps.append(dict(r=r, hh=hh, kb=kb, has_cur=has_cur, has_prev=has_prev, osb=osb, ores=ores,
                                                  last=(hh == 1 and kb == nkb - 1)))
                    ns = len(steps)

                    def stage_a(i):
                        sp_ = steps[i]
                        r, hh, kb, has_cur, has_prev = sp_["r"], sp_["hh"], sp_["kb"], sp_["has_cur"], sp_["has_prev"]
                        h = 2 * hp + hh
                        rows = slice(64 * hh, 64 * hh + 64)
                        bmo = (g * 8 + h) * 256
                        qlo = (kb if has_cur else kb + 1) - qb0
                        qhi = (kb + 1 if has_prev else kb) - qb0
                        ncol = 128 * (qhi - qlo + 1)
                        qc0 = r * nq + 128 * qlo
                        kc0 = r * nk + 128 * kb
                        ps_s = PS[i % 3]
                        ps_sres = "ps%d" % (i % 3)
                        tr.op("pe", lambda: nc.tensor.matmul(
                            ps_s[:, 0:ncol], lhsT=kt[rows, kc0:kc0 + 128], rhs=qt[rows, qc0:qc0 + ncol],
                            start=True, stop=True), reads=[kres, qres], writes=[ps_sres])
                        ef, efres = efs.next()
                        tr.op("act", lambda: nc.scalar.activation(
                            out=ef[:, 0:ncol], in_=ps_s[:, 0:ncol], func=AF.Exp, scale=SCALE),
                            reads=[ps_sres], writes=[efres])
                        em, emres = ems.next()
                        b0 = bmo if has_cur else bmo + 128
                        if kb < qb0:
                            tr.op("dve", lambda: nc.vector.scalar_tensor_tensor(
                                out=em[:, 0:ncol], in0=ef[:, 0:ncol], scalar=hv[:, 0:1], in1=bm[:, b0:b0 + ncol],
                                op0=ALU.mult, op1=ALU.mult), reads=[efres, "bm"], writes=[emres])
                        else:
                            tr.op("dve", lambda: nc.vector.tensor_tensor(
                                out=em[:, 0:ncol], in0=ef[:, 0:ncol], in1=bm[:, b0:b0 + ncol], op=ALU.mult),
                                reads=[efres, "bm"], writes=[emres])
                        sp_["em"] = (em, emres)

                    def stage_b(i):
                        sp_ = steps[i]
                        r, hh, kb, has_cur, has_prev = sp_["r"], sp_["hh"], sp_["kb"], sp_["has_cur"], sp_["has_prev"]
                        osb, ores = sp_["osb"], sp_["ores"]
                        em, emres = sp_["em"]
                        vch = r * nkb + kb
                        col = 0
                        if has_cur:
                            po = PS[3 + (kb % 2)]
                            pores = "ps%d" % (3 + (kb % 2))
                            tr.op("pe", lambda: nc.tensor.matmul(
                                po[:, 0:65], lhsT=em[:, 0:128], rhs=va[:, vch, hh, :], start=False, stop=True),
                                reads=[emres, vres], writes=[pores])
                            qo = kb - qb0
                            evac(osb[:, qo, hh, :], po[:, 0:65], [pores], [ores], eng="dve" if i % 3 else "act")
                            col = 128
                        if has_prev:
                            po = PS[3 + ((kb + 1) % 2)]
                            pores = "ps%d" % (3 + ((kb + 1) % 2))
                            tr.op("pe", lambda: nc.tensor.matmul(
                                po[:, 0:65], lhsT=em[:, col:col + 128], rhs=va[:, vch, hh, :], start=True, stop=False),
                                reads=[emres, vres], writes=[pores])
                        if sp_["last"]:
                            tr.dma("pool", OD[g][r:T:d, 2 * hp:2 * hp + 2, :].rearrange("(qo a) h e -> a qo h e", a=128),
                                   osb[:, 0:nqo, :, :], reads=[ores], writes=[], slot=ores)

                    stage_a(0)
                    if ns > 1:
                        stage_a(1)
                    for i in range(ns):
                        if i + 2 < ns:
                            stage_a(i + 2)
                        stage_b(i)
            tr.barrier()

        if "stop2" in dbg:
            tr.finish()
            return nc

        with contextlib.ExitStack() as p3:
            kas = Rot([sb(p3, "ka%d" % i, [128, VT], BF16) for i in range(2)], "ka")
            qas = Rot([sb(p3, "qa%d" % i, [128, T], BF16) for i in range(2)], "qa")
            vbs_ = Rot([sb(p3, "vv%d" % i, [128, 128, 65], BF16) for i in range(2)], "vv")
            ab = sb(p3, "ab", [128, 8 * NM], F32)
            ct = sb(p3, "ct", [128, 32], F32)
            pm = sb(p3, "pm", [128, 128], F32)
            cb = sb(p3, "cb", [128, 4 * 512], F32)
            vbt = sb(p3, "vbt", [128, 64], F32)
            ksum = sb(p3, "ksum", [64, 64], F32)
            kmb = sb(p3, "kmb", [64, 64], BF16)
            g1s = Rot([sb(p3, "g1%d" % i, [128, 64], F32) for i in range(2)], "g1")
            t8s = Rot([sb(p3, "t8%d" % i, [128, 16], F32) for i in range(2)], "t8")
            sms = Rot([sb(p3, "sm%d" % i, [128, 64], F32) for i in range(2)], "sm")
            mbs = Rot([sb(p3, "mb%d" % i, [128, 128], BF16) for i in range(2)], "mb")
            ets = Rot([sb(p3, "et%d" % i, [128, 512], BF16) for i in range(3)], "et")
            tmps = Rot([sb(p3, "tmpf%d" % i, [128, 512], F32) for i in range(2)], "tmpf")
            otf = sb(p3, "otf", [65, 512], F32)
            obs = Rot([sb(p3, "obst%d" % i, [128, 4, 65], F32) for i in range(2)], "obst")
            for i, (dst, src) in enumerate([(ab, ab_d), (ct, ct_d), (pm, pm_d), (cb, cb_d), (vbt, vb_d)]):
                tr.dma("sp", dst[:], src, writes=["k%d" % i], slot="ld%d" % (i % 2))
            for i, ka in enumerate(kas.tiles):
                tr.dma("sp", ka[64:128, :], sel_d, writes=["ka%d" % i], slot="ld%d" % (i % 2))
            for i, vv in enumerate(vbs_.tiles):
                tr.op("pool", lambda vv=vv: nc.gpsimd.memset(vv[:, :, 64:65], 1.0), writes=["vv%d" % i])
            for i, mb in enumerate(mbs.tiles):
                tr.op("pool", lambda mb=mb: nc.gpsimd.memset(mb[:, 0:64], 0.0), writes=["mb%d" % i])
            tr.barrier()
            otfs = Rot([otf, sb(p3, "otf1", [65, 512], F32)], "otf")
            bufs = {}

            def loads(h):
                ka, kares = kas.next()
                qa, qares = qas.next()
                vv, vvres = vbs_.next()
                bufs[h] = (ka, kares, qa, qares, vv, vvres)
                for part in range(4):
                    tr.dma("sp", ka[0:64, 4096 * part:4096 * part + 4096],
                           KTb[64 * h:64 * h + 64, 4096 * part:4096 * part + 4096],
                           writes=[kares], slot="%sp%d" % (kares, part % 2))
                tr.dma("sp", qa[0:64, :], QTb[64 * h:64 * h + 64, :], writes=[qares + "q"], slot=qares)
                for part in range(8):
                    tr.dma("sp", vv[:, 16 * part:16 * part + 16, 0:64],
                           Vb[2048 * part:2048 * part + 2048, 64 * h:64 * h + 64].rearrange("(c p) e -> p c e", p=128),
                           writes=[vvres], slot="%sp%d" % (vvres, part % 2))

            def gate_tasks(h):
                ka, kares, qa, qares, vv, vvres = bufs[h]
                tasks = []

                def t_kmean():
                    tr.op("dve", lambda: nc.vector.tensor_reduce(
                        out=ksum[:, :], in_=ka[0:64, :].rearrange("p (n k) -> p n k", k=256), axis=AX.X, op=ALU.add),
                        reads=[kares], writes=["ksum"])
                    tr.op("dve", lambda: nc.vector.tensor_copy(out=kmb[:, :], in_=ksum[:, :]), reads=["ksum"], writes=["kmb"])
                tasks.append(t_kmean)
                st = {}
                for qi in range(32):
                    def ta(qi=qi):
                        b_own = (VT - T) // 256 + qi // 2
                        pg = PS[5]
                        tr.op("pe", lambda: nc.tensor.matmul(
                            pg[:, 0:64], lhsT=qa[0:64, 128 * qi:128 * qi + 128], rhs=kmb[:, :], start=True, stop=True),
                            reads=[qares + "q", "kmb"], writes=["ps5"])
                        g1, g1res = g1s.next()
                        tr.op("dve", lambda: nc.vector.tensor_tensor(
                            out=g1[:], in0=pg[:, 0:64], in1=pm[:, 64 - b_own:128 - b_own], op=ALU.add),
                            reads=["ps5"], writes=[g1res])
                        tr.op("dve", lambda: nc.vector.tensor_tensor(out=g1[:], in0=g1[:], in1=vbt[:], op=ALU.add),
                              reads=[g1res], writes=[g1res])
                        t8, t8res = t8s.next()
                        tr.op("dve", lambda: nc.vector.max(out=t8[:, 0:8], in_=g1[:]), reads=[g1res], writes=[t8res])
                        tr.op("dve", lambda: nc.vector.tensor_scalar_max(out=t8[:, 8:9], in0=t8[:, 2:3], scalar1=-1e29),
                              reads=[t8res], writes=[t8res])
                        sm, smres = sms.next()
                        tr.op("dve", lambda: nc.vector.tensor_scalar(
                            out=sm[:], in0=g1[:], scalar1=t8[:, 8:9], scalar2=None, op0=ALU.is_ge),
                            reads=[g1res, t8res], writes=[smres])
                        tr.op("dve", lambda: nc.vector.memset(sm[:, b_own:b_own + 1], 1.0), reads=[smres], writes=[smres])
                        mb, mbres = mbs.next()
                        cti = 4 * h + qi % 4
                        tr.op("dve", lambda: nc.vector.tensor_scalar(
                            out=mb[:, 64:128], in0=sm[:], scalar1=ct[:, cti:cti + 1], scalar2=MBNEG, op0=ALU.mult, op1=ALU.add),
                            reads=[smres], writes=[mbres])
                        st[qi] = (mb, mbres)

                    def tb(qi=qi):
                        mb, mbres = st[qi]
                        pb = PB[qi % 2]
                        pbres = "pb%d" % (qi % 2)
                        tr.op("pe", lambda: nc.tensor.transpose(out=pb[:, 0:128], in_=mb[:, :], identity=ident[:]),
                              reads=[mbres], writes=[pbres])
                        tr.op("dve", lambda: nc.vector.tensor_copy(
                            out=qa[64:128, 128 * qi:128 * qi + 128], in_=pb[64:128, 0:128]),
                            reads=[pbres], writes=[qares + "m%d" % (qi // 4)])
                    tasks.append(ta)
                    tasks.append(tb)
                order = [tasks[0], tasks[1]]
                for qi in range(32):
                    if qi + 1 < 32:
                        order.append(tasks[1 + 2 * (qi + 1)])
                    order.append(tasks[2 + 2 * qi])
                return order

            def main_loop(h, pending):
                ka, kares, qa, qares, vv, vvres = bufs[h]
                items = []
                firsts = {}
                for qt_ in range(8):
                    nch = (VT - T) // 128 + 4 * (qt_ + 1)
                    c_lo = 0
                    if SKIP_FAR:
                        slope = 2.0 ** (-(h + 1))
                        dmin = SKIP_NATS / slope
                        t0q = (VT - T) + 512 * qt_
                        c_lo = max(0, int(np.floor((t0q - 127 - dmin) / 128.0)) + 1)
                    firsts[qt_] = c_lo
                    for c in range(c_lo, nch):
                        items.append((qt_, c, nch))
                n = len(items)
                ets_ = {}
                deferred = {}
                every = max(1, (n - 40) // max(1, len(pending)))

                def stage_a(i):
                    qt_, c, nch = items[i]
                    mi = c - 4 * qt_ + 28
                    ps_s = PS[i % 3]
                    ps_sres = "ps%d" % (i % 3)
                    tr.op("pe", lambda: nc.tensor.matmul(
                        ps_s[:, :], lhsT=ka[:, 128 * c:128 * c + 128], rhs=qa[:, 512 * qt_:512 * qt_ + 512],
                        start=True, stop=True), reads=[kares, qares + "q", qares + "m%d" % qt_], writes=[ps_sres])
                    et, etres = ets.next()
                    ets_[i] = (et, etres)
                    bias = ab[:, h * NM + mi:h * NM + mi + 1]
                    dc = c - (nch - 4)
                    if dc < 0:
                        tr.op("act", lambda: nc.scalar.activation(
                            out=et[:], in_=ps_s[:, :], func=AF.Exp, bias=bias, scale=SCALE),
                            reads=[ps_sres], writes=[etres])
                    else:
                        tmpf, tres = tmps.next()
                        tr.op("dve", lambda: nc.vector.tensor_tensor(
                            out=tmpf[:], in0=ps_s[:, :], in1=cb[:, 512 * dc:512 * dc + 512], op=ALU.add),
                            reads=[ps_sres], writes=[tres])
                        tr.op("act", lambda: nc.scalar.activation(
                            out=et[:], in_=tmpf[:], func=AF.Exp, bias=bias, scale=SCALE),
                            reads=[tres], writes=[etres])

                def stage_b(i):
                    qt_, c, nch = items[i]
                    et, etres = ets_.pop(i)
                    po = PS[3 + qt_ % 2]
                    pores = "ps%d" % (3 + qt_ % 2)
                    tr.op("pe", lambda: nc.tensor.matmul(
                        po[0:65, :], lhsT=vv[:, c, :], rhs=et[:], start=(c == firsts[qt_]), stop=(c == nch - 1)),
                        reads=[etres, vvres], writes=[pores])
                    if c == nch - 1:
                        of, ofres = otfs.next()
                        tr.op("dve", lambda: nc.vector.tensor_copy(out=of[:, :], in_=po[0:65, :]), reads=[pores], writes=[ofres])

                        def fin(qt_=qt_, of=of, ofres=ofres):
                            px = PS[5]
                            for s in range(4):
                                tr.op("pe", lambda s=s: nc.tensor.transpose(
                                    out=px[:, 65 * s:65 * s + 65], in_=of[0:65, 128 * s:128 * s + 128],
                                    identity=identf[0:65, 0:65]), reads=[ofres], writes=["ps5"])
                            obst, obres = obs.next()
                            tr.op("dve", lambda: nc.vector.tensor_copy(
                                out=obst[:].rearrange("p s e -> p (s e)"), in_=px[:, 0:260]), reads=["ps5"], writes=[obres])
                            tr.dma("pool", OB[512 * qt_:512 * qt_ + 512, h, :].rearrange("(s p) e -> p s e", p=128), obst[:],
                                   reads=[obres], writes=[], slot=obres)
                        deferred[min(n - 1, i + 4)] = fin

                stage_a(0)
                stage_a(1)
                for i in range(n):
                    if i + 2 < n:
                        stage_a(i + 2)
                    stage_b(i)
                    if i in deferred:
                        deferred.pop(i)()
                    if pending and i >= 8 and (i - 8) % every == 0:
                        pending.pop(0)()
                for k in sorted(deferred):
                    deferred[k]()
                while pending:
                    pending.pop(0)()

            horder = [7, 6, 5, 4, 3, 2, 1, 0]
            loads(horder[0])
            for t in gate_tasks(horder[0]):
                t()
            for hi, h in enumerate(horder):
                pending = []
                if hi + 1 < 8:
                    loads(horder[hi + 1])
                    pending = gate_tasks(horder[hi + 1])
                main_loop(h, pending)
            tr.barrier()

        if "stop3" in dbg:
            tr.finish()
            return nc

        with contextlib.ExitStack() as p4:
            wst = sb(p4, "wst4", [128, 8, 512], F32)
            wkv = sb(p4, "wkv", [128, 8, 512], BF16)
            mx = sb(p4, "mx", [128, 2, D], F32)
            mhb = sb(p4, "mhb", [128, 2, D], BF16)
            junk = sb(p4, "junk4", [128, D], BF16)
            mss = sb(p4, "mss", [128, 6], F32)
            memT = sb(p4, "memT", [128, 8, 256], BF16)
            mkT = sb(p4, "mkT", [128, 2, 256], BF16)
            mva = sb(p4, "mva", [128, 2, 4, 65], BF16)
            mq = sb(p4, "mq", [128, 2, T], BF16)
            ems = Rot([sb(p4, "em4%d" % i, [128, 2, 512], BF16) for i in range(2)], "em4")
            rds = Rot([sb(p4, "rd4%d" % i, [128, 4], F32) for i in range(2)], "rd4")
            oms = Rot([sb(p4, "om4%d" % i, [128, 4, 64], F32) for i in range(2)], "om4")
            tr.dma("sp", wst[:], w_kv.rearrange("(fc p) c -> p fc c", p=128), writes=["wst4"], slot="ld0")
            tr.dma("sp", mx[:], memb.rearrange("(s p) f -> p s f", p=128), writes=["mx"], slot="ld1")
            tr.dma("sp", mq[:], MQT.rearrange("(pr p) t -> p pr t", p=128), writes=["mq"], slot="ld0")
            tr.op("pool", lambda: nc.gpsimd.memset(mva[:, :, :, 64:65], 1.0), writes=["mva1"])
            for fc in range(8):
                conv_scale(wkv[:, fc, :], wst[:, fc, :], mnw[:, fc:fc + 1], ["wst4"], ["wkv"])
            rms_tile({"junk": junk}, mx, "mx", 2, mhb, "mhb", mss, "mss")
            for fc in range(8):
                pb = PB[fc % 2]
                pbres = "pb%d" % (fc % 2)
                for s in range(2):
                    tr.op("pe", lambda s=s, fc=fc, pb=pb: nc.tensor.transpose(
                        out=pb[:, s * 128:(s + 1) * 128], in_=mhb[:, s, fc * 128:(fc + 1) * 128], identity=ident[:]),
                        reads=["mhb"], writes=[pbres])
                evac(memT[:, fc, :], pb[:, 0:256], [pbres], ["memT"])
            for pr in range(2):
                ps = PS[pr]
                for fc in range(8):
                    tr.op("pe", lambda fc=fc, pr=pr, ps=ps: nc.tensor.matmul(
                        ps[:, 0:256], lhsT=wkv[:, fc, pr * 128:(pr + 1) * 128], rhs=memT[:, fc, :],
                        start=(fc == 0), stop=(fc == 7)), reads=["wkv", "memT"], writes=["ps%d" % pr])
                evac(mkT[:, pr, :], ps[:, 0:256], ["ps%d" % pr], ["mkT"])
            for s in range(2):
                ps = PS[2 + s]
                for fc in range(8):
                    tr.op("pe", lambda fc=fc, s=s, ps=ps: nc.tensor.matmul(
                        ps[:, 0:256], lhsT=memT[:, fc, s * 128:(s + 1) * 128], rhs=wkv[:, fc, 256:512],
                        start=(fc == 0), stop=(fc == 7)), reads=["wkv", "memT"], writes=["ps%d" % (2 + s)])
                evac(mva[:, s, :, 0:64], ps[:, 0:256].rearrange("p (h e) -> p h e", e=64), ["ps%d" % (2 + s)], ["mva"])
            it = 0
            for h in range(4):
                pr = h // 2
                rows = slice(64 * (h % 2), 64 * (h % 2) + 64)
                for qt_ in range(8):
                    em, emres = ems.next()
                    for mc in range(2):
                        ps_s = PS[mc]
                        tr.op("pe", lambda ps_s=ps_s, mc=mc, qt_=qt_, pr=pr, rows=rows: nc.tensor.matmul(
                            ps_s[:, :], lhsT=mkT[rows, pr, 128 * mc:128 * mc + 128], rhs=mq[rows, pr, 512 * qt_:512 * qt_ + 512],
                            start=True, stop=True), reads=["mkT", "mq"], writes=["ps%d" % mc])
                        tr.op("act", lambda em=em, ps_s=ps_s, mc=mc: nc.scalar.activation(
                            out=em[:, mc, :], in_=ps_s[:, :], func=AF.Exp, scale=SCALE), reads=["ps%d" % mc], writes=[emres])
                    po = PS[2 + it % 2]
                    pores = "ps%d" % (2 + it % 2)
                    it += 1
                    for s in range(4):
                        for mc in range(2):
                            tr.op("pe", lambda po=po, s=s, mc=mc, em=em, h=h: nc.tensor.matmul(
                                po[:, 65 * s:65 * s + 65], lhsT=em[:, mc, 128 * s:128 * s + 128], rhs=mva[:, mc, h, :],
                                start=(mc == 0), stop=(mc == 1)), reads=[emres, "mva", "mva1"], writes=[pores])
                    rd, rdres = rds.next()
                    pov = po[:, 0:260].rearrange("p (s e) -> p s e", e=65)
                    tr.op("dve", lambda rd=rd, pov=pov: nc.vector.reciprocal(out=rd[:], in_=pov[:, :, 64]),
                          reads=[pores], writes=[rdres])
                    om, omres = oms.next()
                    tr.op("dve", lambda om=om, rd=rd, pov=pov: nc.vector.tensor_tensor(
                        out=om[:], in0=pov[:, :, 0:64], in1=rd[:].unsqueeze(2).to_broadcast([128, 4, 64]), op=ALU.mult),
                        reads=[pores, rdres], writes=[omres])
                    tr.dma("pool", OM[512 * qt_:512 * qt_ + 512, 64 * h:64 * h + 64].rearrange("(s p) e -> p s e", p=128),
                           om[:], reads=[omres], writes=[], slot=omres)
            tr.barrier()

        if "stop4" in dbg:
            tr.finish()
            return nc

        with contextlib.ExitStack() as p5:
            wst = sb(p5, "wst5", [128, 2, D], F32)
            wbr = sb(p5, "wbr", [128, 10, D], BF16)
            wout = sb(p5, "wout", [128, 8, D], BF16)
            fnw = sb(p5, "fnw", [128, D], F32)
            tr.dma("sp", fnw[:], fnw_d, writes=["fnw"], slot="ld1")
            for (src, nfc, f0, dstw) in ((w_ba, 4, 0, wbr), (w_bb, 4, 4, wbr), (w_bm, 2, 8, wbr), (w_o, 8, 0, wout)):
                for f2 in range(0, nfc, 2):
                    tr.dma("sp", wst[:], src[128 * f2:128 * f2 + 256, :].rearrange("(fc p) c -> p fc c", p=128),
                           writes=["wst5"], slot="ld0")
                    for k in range(2):
                        conv_scale(dstw[:, f0 + f2 + k, :], wst[:, k, :], None, ["wst5"], ["wbr"])
            ods = Rot([sb(p5, "od%d" % i, [128, 3, 8, 65], F32) for i in range(2)], "od")
            obl = Rot([sb(p5, "obl%d" % i, [128, 8, 65], F32) for i in range(2)], "obl")
            oml = Rot([sb(p5, "oml%d" % i, [128, 256], F32) for i in range(2)], "oml")
            gts = Rot([sb(p5, "gt%d" % i, [128, 1280], BF16) for i in range(2)], "gt")
            rdn = Rot([sb(p5, "rdn%d" % i, [128, 16], F32) for i in range(2)], "rdn")
            oas = Rot([sb(p5, "oa%d" % i, [128, 1280], F32) for i in range(2)], "oa")
            ogs = Rot([sb(p5, "og%d" % i, [128, 1280], BF16) for i in range(2)], "og")
            ogT = Rot([sb(p5, "ogT%d" % i, [128, 10, 512], BF16) for i in range(2)], "ogT")
            sgs = Rot([sb(p5, "sg%d" % i, [128, 24, 512], BF16) for i in range(2)], "sg")
            m1s = Rot([sb(p5, "m1%d" % i, [128, 512], F32) for i in range(2)], "m1")
            m2s = Rot([sb(p5, "m2%d" % i, [128, 512], F32) for i in range(2)], "m2")
            m3s = Rot([sb(p5, "m3%d" % i, [128, 512], F32) for i in range(2)], "m3")
            mTs = Rot([sb(p5, "mT%d" % i, [128, 8, 512], BF16) for i in range(2)], "mT")
            xrs = Rot([sb(p5, "xr%d" % i, [128, D], F32) for i in range(2)], "xr")
            ys = Rot([sb(p5, "y%d" % i, [128, D], F32) for i in range(2)], "y")
            junk = sb(p5, "junk5", [128, D], BF16)
            sss = Rot([sb(p5, "ss5%d" % i, [128, 4], F32) for i in range(2)], "ss5")
            pbi = 0
            p5st = {}

            def stage_a5(tt):
                nonlocal pbi
                ogt, ogtres = ogT.next()
                sg, sgres = sgs.next()
                tr.dma("sp", sg[:], SG[:, 512 * tt:512 * tt + 512].rearrange("(c p) t -> p c t", p=128), writes=[sgres], slot=sgres)
                for s in range(4):
                    tok0 = 512 * tt + 128 * s
                    od, odres = ods.next()
                    for g in range(3):
                        tr.dma("sp", od[:, g, :, :], OD[g][tok0:tok0 + 128, :, :], writes=[odres + "g%d" % g],
                               slot="%sg%d" % (odres, g))
                    ob, obres = obl.next()
                    tr.dma("sp", ob[:], OB[tok0:tok0 + 128, :, :], writes=[obres], slot=obres)
                    oml_, omres = oml.next()
                    tr.dma("sp", oml_[:], OM[tok0:tok0 + 128, :], writes=[omres], slot=omres)
                    gt, gtres = gts.next()
                    tr.dma("sp", gt[:, 0:512], Ga[tok0:tok0 + 128, :], writes=[gtres + "a"], slot=gtres + "a")
                    tr.dma("sp", gt[:, 512:1024], Gb[tok0:tok0 + 128, :], writes=[gtres + "b"], slot=gtres + "b")
                    tr.dma("sp", gt[:, 1024:1280], Gm[tok0:tok0 + 128, :], writes=[gtres + "m"], slot=gtres + "m")
                    tr.op("dve", lambda od=od: nc.vector.tensor_tensor(out=od[:, 0], in0=od[:, 0], in1=od[:, 1], op=ALU.add),
                          reads=[odres + "g0", odres + "g1"], writes=[odres + "g0"])
                    tr.op("pool", lambda od=od: nc.gpsimd.tensor_tensor(out=od[:, 0], in0=od[:, 0], in1=od[:, 2], op=ALU.add),
                          reads=[odres + "g0", odres + "g2"], writes=[odres + "g0"])
                    rd, rdres = rdn.next()
                    tr.op("dve", lambda rd=rd, od=od: nc.vector.reciprocal(out=rd[:, 0:8], in_=od[:, 0, :, 64]),
                          reads=[odres + "g0"], writes=[rdres])
                    tr.op("dve", lambda rd=rd, ob=ob: nc.vector.reciprocal(out=rd[:, 8:16], in_=ob[:, :, 64]),
                          reads=[obres, rdres], writes=[rdres])
                    oa, oares = oas.next()
                    tr.op("dve", lambda oa=oa, od=od, rd=rd: nc.vector.tensor_tensor(
                        out=oa[:, 0:512].rearrange("p (h e) -> p h e", e=64), in0=od[:, 0, :, 0:64],
                        in1=rd[:, 0:8].unsqueeze(2).to_broadcast([128, 8, 64]), op=ALU.mult),
                        reads=[odres + "g0", rdres], writes=[oares])
                    tr.op("pool", lambda oa=oa, ob=ob, rd=rd: nc.gpsimd.tensor_tensor(
                        out=oa[:, 512:1024].rearrange("p (h e) -> p h e", e=64), in0=ob[:, :, 0:64],
                        in1=rd[:, 8:16].unsqueeze(2).to_broadcast([128, 8, 64]), op=ALU.mult),
                        reads=[obres, rdres, oares], writes=[oares])
                    og, ogres = ogs.next()
                    tr.op("dve", lambda og=og, oa=oa, gt=gt: nc.vector.tensor_tensor(
                        out=og[:, 0:1024], in0=oa[:, 0:1024], in1=gt[:, 0:1024], op=ALU.mult),
                        reads=[oares, gtres + "a", gtres + "b"], writes=[ogres])
                    tr.op("pool", lambda og=og, oml_=oml_, gt=gt: nc.gpsimd.tensor_tensor(
                        out=og[:, 1024:1280], in0=oml_[:], in1=gt[:, 1024:1280], op=ALU.mult),
                        reads=[omres, gtres + "m", ogres], writes=[ogres])
                    for fc2 in range(0, 10, 2):
                        pb = PB[pbi % 2]
                        pbres = "pb%d" % (pbi % 2)
                        pbi += 1
                        for k in range(2):
                            fc = fc2 + k
                            tr.op("pe", lambda pb=pb, og=og, fc=fc, k=k: nc.tensor.transpose(
                                out=pb[:, 128 * k:128 * k + 128], in_=og[:, 128 * fc:128 * fc + 128], identity=ident[:]),
                                reads=[ogres], writes=[pbres])
                        evac(ogt[:, fc2:fc2 + 2, 128 * s:128 * s + 128], pb[:, 0:256].rearrange("p (k t) -> p k t", t=128),
                             [pbres], [ogtres])
                p5st[tt] = (ogt, ogtres, sg, sgres)

            def stage_b5(tt):
                ogt, ogtres, sg, sgres = p5st.pop(tt)
                mT, mTres = mTs.next()
                for cc in range(8):
                    pa, pbk, pm_ = PS[0], PS[1], PS[2]
                    for (ps, pres, f0, nf) in ((pa, "ps0", 0, 4), (pbk, "ps1", 4, 4), (pm_, "ps2", 8, 2)):
                        for k in range(nf):
                            tr.op("pe", lambda ps=ps, f0=f0, k=k, nf=nf, cc=cc: nc.tensor.matmul(
                                ps[:, :], lhsT=wbr[:, f0 + k, 128 * cc:128 * cc + 128], rhs=ogt[:, f0 + k, :],
                                start=(k == 0), stop=(k == nf - 1)), reads=[ogtres, "wbr"], writes=[pres])
                    m1, m1res = m1s.next()
                    m2, m2res = m2s.next()
                    m3, m3res = m3s.next()
                    tr.op("dve", lambda m1=m1, cc=cc: nc.vector.tensor_tensor(out=m1[:], in0=pa[:, :], in1=sg[:, cc, :], op=ALU.mult),
                          reads=["ps0", sgres], writes=[m1res])
                    tr.op("dve", lambda m2=m2, cc=cc: nc.vector.tensor_tensor(out=m2[:], in0=pbk[:, :], in1=sg[:, 8 + cc, :], op=ALU.mult),
                          reads=["ps1", sgres], writes=[m2res])
                    tr.op("dve", lambda m3=m3, cc=cc: nc.vector.tensor_tensor(out=m3[:], in0=pm_[:, :], in1=sg[:, 16 + cc, :], op=ALU.mult),
                          reads=["ps2", sgres], writes=[m3res])
                    tr.op("pool", lambda m1=m1, m2=m2: nc.gpsimd.tensor_tensor(out=m1[:], in0=m1[:], in1=m2[:], op=ALU.add),
                          reads=[m1res, m2res], writes=[m1res])
                    tr.op("pool", lambda m1=m1, m3=m3, mT=mT, cc=cc: nc.gpsimd.tensor_tensor(
                        out=mT[:, cc, :], in0=m1[:], in1=m3[:], op=ALU.add), reads=[m1res, m3res], writes=[mTres])
                for s in range(4):
                    tok0 = 512 * tt + 128 * s
                    xr, xrres = xrs.next()
                    tr.dma("sp", xr[:], xs[VT - T + tok0:VT - T + tok0 + 128, :], writes=[xrres], slot=xrres)
                    y, yres = ys.next()
                    for half in range(2):
                        ps = PS[3 + half]
                        pres = "ps%d" % (3 + half)
                        for fc in range(8):
                            tr.op("pe", lambda ps=ps, fc=fc, s=s, half=half: nc.tensor.matmul(
                                ps[:, :], lhsT=mT[:, fc, 128 * s:128 * s + 128], rhs=wout[:, fc, 512 * half:512 * half + 512],
                                start=(fc == 0), stop=(fc == 7)), reads=[mTres, "wout"], writes=[pres])
                        tr.op("dve", lambda ps=ps, y=y, xr=xr, half=half: nc.vector.tensor_tensor(
                            out=y[:, 512 * half:512 * half + 512], in0=ps[:, :], in1=xr[:, 512 * half:512 * half + 512], op=ALU.add),
                            reads=[pres, xrres], writes=[yres])
                    ssb, ssres = sss.next()
                    tr.op("pool", lambda ssb=ssb: nc.gpsimd.memset(ssb[:], 0.0), writes=[ssres])
                    tr.op("act", lambda y=y, ssb=ssb: nc.scalar.activation(out=junk[:], in_=y[:], func=AF.Square, accum_out=ssb[:, 0:1]),
                          reads=[yres, ssres], writes=["junk5", ssres])
                    tr.op("act", lambda ssb=ssb: nc.scalar.activation(out=ssb[:, 1:2], in_=ssb[:, 0:1], func=AF.Sqrt, bias=EPS, scale=1.0 / D),
                          reads=[ssres], writes=[ssres])
                    tr.op("dve", lambda ssb=ssb: nc.vector.reciprocal(out=ssb[:, 2:3], in_=ssb[:, 1:2]), reads=[ssres], writes=[ssres])
                    tr.op("dve", lambda y=y, ssb=ssb: nc.vector.scalar_tensor_tensor(
                        out=y[:], in0=y[:], scalar=ssb[:, 2:3], in1=fnw[:], op0=ALU.mult, op1=ALU.mult),
                        reads=[yres, ssres, "fnw"], writes=[yres])
                    tr.dma("pool", out_d[tok0:tok0 + 128, :], y[:], reads=[yres], writes=[], slot=yres)

            stage_a5(0)
            for tt in range(8):
                if tt + 1 < 8:
                    stage_a5(tt + 1)
                stage_b5(tt)
        tr.finish()
    build_program.stats = (dict(tr.cnt), tr.nsem, tr.nwait, tr.ndma)
    return nc


def _const_tables():
    bf = ml_dtypes.bfloat16
    slopes = (2.0 ** (-8.0 * np.arange(1, 17) / 16)).astype(np.float32)
    sa = slopes[0::2].astype(np.float64)
    sbm = slopes[1::2].astype(np.float64)
    k = np.arange(128)[:, None].astype(np.float64)
    a = np.arange(128)[None, :].astype(np.float64)
    bm = np.zeros((128, 3, 8, 256), np.float32)
    for g, d in enumerate(DILS):
        for h in range(8):
            cur = np.where(k <= a, np.exp(-sa[h] * d * (a - k)), 0.0)
            prev = np.where(k >= a, np.exp(-sa[h] * d * (a + 128 - k)), 0.0)
            bm[:, g, h, 0:128] = cur
            bm[:, g, h, 128:256] = prev
    sel = (np.arange(VT)[None, :] // 256 == np.arange(64)[:, None]).astype(np.float32).astype(bf)
    p = np.arange(128)[:, None].astype(np.float64)
    m = (np.arange(NM)[None, :] - 124).astype(np.float64)
    ab = np.zeros((128, 8, NM), np.float32)
    for h in range(8):
        ab[:, h, :] = sbm[h] * (p + 128 * m)
    ct = np.zeros((128, 8, 4), np.float32)
    for h in range(8):
        for s in range(4):
            ct[:, h, s] = -MBNEG - 8.0 * sbm[h] * (p[:, 0] + 128 * s)
    pm = np.concatenate([np.zeros((128, 64), np.float32), np.full((128, 64), NEG, np.float32)], axis=1)
    cb = np.zeros((128, 4, 512), np.float32)
    tl = np.arange(512)[None, :]
    for c in range(4):
        kp = 128 * c + np.arange(128)[:, None]
        same = (tl // 256) == (c // 2)
        cb[:, c, :] = np.where(same & (kp > tl), -8e5, 0.0)
    return dict(
        ident=np.eye(128, dtype=np.float32).astype(bf), identf=np.eye(128, dtype=np.float32),
        bm=bm.reshape(128, -1), sel=sel, ab=ab.reshape(128, -1), ct=ct.reshape(128, -1), pm=pm,
        cb=cb.reshape(128, -1))


def make_in_maps(x, mem, norm_w, mem_norm_w, w_in, b_merge, w_mem_kv, w_branch_a, w_branch_b, w_branch_m, w_out,
                 final_norm_w):
    f = lambda a: np.ascontiguousarray(np.asarray(a, dtype=np.float32))
    consts = _const_tables()
    shared = dict(
        w_in=f(w_in[0]), w_mem_kv=f(w_mem_kv[0]), w_branch_a=f(w_branch_a[0]), w_branch_b=f(w_branch_b[0]),
        w_branch_m=f(w_branch_m[0]), w_out=f(w_out[0]),
        nw=f(np.asarray(norm_w[0]).reshape(8, 128).T), mnw=f(np.asarray(mem_norm_w[0]).reshape(8, 128).T),
        bmg=f(np.asarray(b_merge[0]).reshape(24, 128).T),
        fnw=f(np.broadcast_to(np.asarray(final_norm_w)[None, :], (128, D))), **consts)
    x = np.asarray(x)
    mem = np.asarray(mem)
    in_maps = []
    for c in range(8):
        b, j = c // 4, c % 4
        xsv = np.zeros((VT, D), np.float32)
        n = (j + 1) * T
        xsv[VT - n:] = x[b, :n]
        vbv = np.where(np.arange(64) >= (3 - j) * 16, 0.0, NEG).astype(np.float32)
        m = dict(shared)
        m.update(xs=xsv, memb=f(mem[b]), vb=f(np.broadcast_to(vbv[None, :], (128, 64))),
                 hv=np.full((128, 1), 1.0 if j > 0 else 0.0, np.float32))
        in_maps.append(m)
    return in_maps


def kernel(**inputs):
    in_maps = make_in_maps(**inputs)
    nc = build_program()
    res = run_bass_kernel_spmd(nc, in_maps, core_ids=list(range(8)))
    out = np.zeros((2, 4 * T, D), np.float32)
    for c in range(8):
        b, j = c // 4, c % 4
        out[b, j * T:(j + 1) * T] = res.results[c]["out"]
    return out
```

```python
import contextlib
import numpy as np
import ml_dtypes
import concourse.bass as bass
import concourse.mybir as mybir
from concourse.bass_utils import run_bass_kernel_spmd

F32 = mybir.dt.float32
BF16 = mybir.dt.bfloat16
AF = mybir.ActivationFunctionType
ALU = mybir.AluOpType
AX = mybir.AxisListType

D = 1024
T = 4096
VT = 16384
HALO = 2048
HT = HALO + T
DIN = 10752
DILS = (1, 4, 16)
SCALE = 0.125
EPS = 1e-6
NEG = -1e30
MBNEG = -30000.0
NM = 160
SKIP_FAR = True
SKIP_NATS = 164.0


class Tr:
    EPOCH = 16000
    DEPOCH = 1000

    def __init__(self, nc):
        self.nc = nc
        self.eng = {"pe": nc.tensor, "act": nc.scalar, "dve": nc.vector, "pool": nc.gpsimd, "sp": nc.sync}
        self.cnt = {e: 0 for e in ("pe", "act", "dve", "pool")}
        self.esem = {e: [] for e in self.cnt}
        self.slot = {}
        self.lastw = {}
        self.readers = {}
        self.waited = {e: {} for e in self.eng}
        self.nsem = 0
        self.nwait = 0
        self.ndma = 0
        self.sems = {}
        self.free_slots = []

    def _newsem(self, name):
        s = self.nc.alloc_semaphore(name)
        self.nsem += 1
        self.sems[id(s)] = s
        return s

    @staticmethod
    def _add(deps, ev):
        k = id(ev[0])
        if k not in deps or deps[k][1] < ev[1]:
            deps[k] = ev

    def _deps(self, reads, writes):
        deps = {}
        for r in reads:
            if r in self.lastw:
                self._add(deps, self.lastw[r])
        for w in writes:
            if w in self.lastw:
                self._add(deps, self.lastw[w])
            for ev in self.readers.get(w, {}).values():
                self._add(deps, ev)
        return deps

    def _emit_waits(self, eng, deps):
        e = self.eng[eng]
        wd = self.waited[eng]
        for k, (sem, val, peng) in deps.items():
            if peng == eng and eng == "pe":
                continue
            if wd.get(k, 0) >= val:
                continue
            e.wait_ge(sem, val)
            self.nwait += 1
            wd[k] = val

    def _record(self, ev, reads, writes):
        for w in writes:
            self.lastw[w] = ev
            self.readers[w] = {}
        for r in reads:
            if r in writes:
                continue
            self._add(self.readers.setdefault(r, {}), ev)

    def op(self, eng, fn, reads=(), writes=()):
        deps = self._deps(reads, writes)
        self._emit_waits(eng, deps)
        ins = fn()
        n = self.cnt[eng]
        ep = n // self.EPOCH
        if ep >= len(self.esem[eng]):
            self.esem[eng].append(self._newsem("e_%s_%d" % (eng, ep)))
        sem = self.esem[eng][ep]
        val = n % self.EPOCH + 1
        ins.then_inc(sem, 1)
        self.cnt[eng] = n + 1
        ev = (sem, val, eng)
        self._record(ev, reads, writes)
        return ev

    def dma(self, q, out, in_, reads=(), writes=(), slot=None):
        deps = self._deps(reads, writes)
        st = self.slot.get(slot)
        if st is None and self.free_slots:
            st = self.free_slots.pop()
            self.slot[slot] = st
        if st is None or st[1] >= self.DEPOCH:
            if st is not None:
                self._add(deps, (st[0], 16 * st[1], "dma"))
            st = [self._newsem("d_%s" % slot), 0]
            self.slot[slot] = st
        elif st[1] > 0:
            self._add(deps, (st[0], 16 * st[1], "dma"))
        self._emit_waits(q, deps)
        ins = self.eng[q].dma_start(out=out, in_=in_)
        st[1] += 1
        self.ndma += 1
        ins.then_inc(st[0], 16)
        ev = (st[0], 16 * st[1], "dma")
        self._record(ev, reads, writes)
        return ev

    def _all_events(self):
        deps = {}
        for e, n in self.cnt.items():
            if n > 0:
                ep = (n - 1) // self.EPOCH
                self._add(deps, (self.esem[e][ep], (n - 1) % self.EPOCH + 1, "x"))
        for st in list(self.slot.values()) + self.free_slots:
            if st[1] > 0:
                self._add(deps, (st[0], 16 * st[1], "dma"))
        return deps

    def barrier(self):
        deps = self._all_events()
        for e in self.eng:
            self._emit_waits(e, dict(deps))
        self.lastw = {}
        self.readers = {}
        self.free_slots.extend(self.slot.values())
        self.slot = {}

    def finish(self):
        self._emit_waits("sp", self._all_events())


class Rot:
    def __init__(self, tiles, name):
        self.tiles = tiles
        self.name = name
        self.i = 0

    def next(self):
        k = self.i % len(self.tiles)
        self.i += 1
        return self.tiles[k], "%s%d" % (self.name, k)


def build_program(dbg=()):
    nc = bass.Bass("TRN2", target_bir_lowering=False)
    tr = Tr(nc)

    def din(name, shape, dt=F32):
        return nc.dram_tensor(name, list(shape), dt, kind="ExternalInput").ap()

    def dscr(name, shape, dt):
        kind = "ExternalOutput" if name in dbg else "Internal"
        return nc.dram_tensor(name, list(shape), dt, kind=kind).ap()

    xs = din("xs", [VT, D])
    memb = din("memb", [256, D])
    w_in = din("w_in", [D, DIN])
    w_kv = din("w_mem_kv", [D, 512])
    w_ba = din("w_branch_a", [512, D])
    w_bb = din("w_branch_b", [512, D])
    w_bm = din("w_branch_m", [256, D])
    w_o = din("w_out", [D, D])
    nw_d = din("nw", [128, 8])
    mnw_d = din("mnw", [128, 8])
    bmg_d = din("bmg", [128, 24])
    fnw_d = din("fnw", [128, D])
    ident_d = din("ident", [128, 128], BF16)
    identf_d = din("identf", [128, 128])
    bm_d = din("bm", [128, 3 * 8 * 256])
    sel_d = din("sel", [64, VT], BF16)
    ab_d = din("ab", [128, 8 * NM])
    ct_d = din("ct", [128, 32])
    pm_d = din("pm", [128, 128])
    cb_d = din("cb", [128, 4 * 512])
    vb_d = din("vb", [128, 64])
    hv_d = din("hv", [128, 1])
    out_d = nc.dram_tensor("out", [T, D], F32, kind="ExternalOutput").ap()

    KTb = dscr("KTb", [512, VT], BF16)
    Vb = dscr("Vb", [VT, 512], BF16)
    QTb = dscr("QTb", [512, T], BF16)
    QTg = [dscr("QTg%d" % g, [512, T], BF16) for g in range(3)]
    KTg = [dscr("KTg%d" % g, [512, HT], BF16) for g in range(3)]
    Vg = [dscr("Vg%d" % g, [HT, 512], BF16) for g in range(3)]
    Ga = dscr("Ga", [T, 512], BF16)
    Gb = dscr("Gb", [T, 512], BF16)
    Gm = dscr("Gm", [T, 256], BF16)
    MQT = dscr("MQT", [256, T], BF16)
    SG = dscr("SG", [3072, T], BF16)
    OD = [dscr("OD%d" % g, [T, 8, 65], F32) for g in range(3)]
    OB = dscr("OB", [T, 8, 65], F32)
    OM = dscr("OM", [T, 256], F32)

    es = contextlib.ExitStack()

    def sb(stack, name, shape, dt):
        return stack.enter_context(nc.sbuf_tensor("s_" + name, list(shape), dt))

    with es:
        ident = sb(es, "ident", [128, 128], BF16)
        identf = sb(es, "identf", [128, 128], F32)
        nw = sb(es, "nw", [128, 8], F32)
        mnw = sb(es, "mnw", [128, 8], F32)
        hv = sb(es, "hv", [128, 1], F32)
        PS = [es.enter_context(nc.psum_tensor("ps%d" % i, [128, 512], F32)) for i in range(6)]
        PB = [es.enter_context(nc.psum_tensor("pb%d" % i, [128, 1024], BF16)) for i in range(2)]
        for i, (dst, src) in enumerate([(ident, ident_d), (identf, identf_d), (nw, nw_d), (mnw, mnw_d), (hv, hv_d)]):
            tr.dma("sp", dst[:], src, writes=["c%d" % i], slot="ld%d" % (i % 2))
        tr.barrier()

        evac_i = [0]

        def evac(out, in_, rd, wr, func=None, bias=None, eng=None):
            if func is not None:
                e = "act"
            elif eng is not None:
                e = eng
            else:
                e = "act" if evac_i[0] % 2 == 0 else "dve"
                evac_i[0] += 1
            if e == "act":
                if func is None:
                    tr.op("act", lambda: nc.scalar.copy(out=out, in_=in_), reads=rd, writes=wr)
                elif bias is None:
                    tr.op("act", lambda: nc.scalar.activation(out=out, in_=in_, func=func), reads=rd, writes=wr)
                else:
                    tr.op("act", lambda: nc.scalar.activation(out=out, in_=in_, func=func, bias=bias), reads=rd, writes=wr)
            else:
                tr.op("dve", lambda: nc.vector.tensor_copy(out=out, in_=in_), reads=rd, writes=wr)

        cv_i = [0]

        def conv_scale(out, in_, sc, rd, wr):
            e = ("dve", "pool", "act")[cv_i[0] % 3]
            cv_i[0] += 1
            if e == "act":
                if sc is None:
                    tr.op("act", lambda: nc.scalar.copy(out=out, in_=in_), reads=rd, writes=wr)
                else:
                    tr.op("act", lambda: nc.scalar.activation(out=out, in_=in_, func=AF.Copy, scale=sc), reads=rd, writes=wr)
            else:
                en = nc.vector if e == "dve" else nc.gpsimd
                if sc is None:
                    tr.op(e, lambda: en.tensor_copy(out=out, in_=in_), reads=rd, writes=wr)
                else:
                    tr.op(e, lambda: en.tensor_scalar(out=out, in0=in_, scalar1=sc, scalar2=None, op0=ALU.mult),
                          reads=rd, writes=wr)

        def rms_tile(st, xt, xres, nsub, hb, hbres, ssb, ssres):
            tr.op("dve", lambda: nc.vector.memset(ssb[:], 0.0), writes=[ssres])
            junk = st["junk"]
            for s in range(nsub):
                tr.op("act", lambda s=s: nc.scalar.activation(out=junk[:], in_=xt[:, s, :], func=AF.Square,
                                                               accum_out=ssb[:, s:s + 1]),
                      reads=[xres, ssres], writes=["junk", ssres])
            tr.op("act", lambda: nc.scalar.activation(out=ssb[:, nsub:2 * nsub], in_=ssb[:, 0:nsub], func=AF.Sqrt,
                                                       bias=EPS, scale=1.0 / D), reads=[ssres], writes=[ssres])
            tr.op("dve", lambda: nc.vector.reciprocal(out=ssb[:, 2 * nsub:3 * nsub], in_=ssb[:, nsub:2 * nsub]),
                  reads=[ssres], writes=[ssres])
            for s in range(nsub):
                sc = ssb[:, 2 * nsub + s:2 * nsub + s + 1]
                if s % 2 == 0:
                    tr.op("dve", lambda s=s, sc=sc: nc.vector.tensor_scalar(out=hb[:, s, :], in0=xt[:, s, :], scalar1=sc,
                                                                           scalar2=None, op0=ALU.mult),
                          reads=[xres, ssres], writes=[hbres])
                else:
                    tr.op("act", lambda s=s, sc=sc: nc.scalar.activation(out=hb[:, s, :], in_=xt[:, s, :], func=AF.Copy, scale=sc),
                          reads=[xres, ssres], writes=[hbres])

        with contextlib.ExitStack() as p1:
            hT = sb(p1, "hT", [128, 8, HT], BF16)
            wst = sb(p1, "wst", [128, 8, 512], F32)
            wbf = [sb(p1, "wbf%d" % i, [128, 8, 512], BF16) for i in range(2)]
            junk = sb(p1, "junk", [128, D], BF16)
            st = {"junk": junk}
            bmg = sb(p1, "bmg", [128, 24], F32)
            tr.dma("sp", bmg[:], bmg_d, writes=["bmg"], slot="ld0")

            def load_w(col0, ncols, dst, dres, src=w_in, scale=nw):
                tr.dma("sp", wst[:, :, 0:ncols], src[:, col0:col0 + ncols].rearrange("(fc p) c -> p fc c", p=128),
                       writes=["wst"], slot="wst")
                for fc in range(8):
                    conv_scale(dst[:, fc, 0:ncols], wst[:, fc, 0:ncols], scale[:, fc:fc + 1], ["wst"], [dres])

            with contextlib.ExitStack() as p1a:
                xts = Rot([sb(p1a, "xt%d" % i, [128, 2, D], F32) for i in range(4)], "xt")
                hbs = Rot([sb(p1a, "hb%d" % i, [128, 2, D], BF16) for i in range(2)], "hb")
                sss = Rot([sb(p1a, "ss%d" % i, [128, 6], F32) for i in range(2)], "ss")
                hts = Rot([sb(p1a, "htt%d" % i, [128, 8, 256], BF16) for i in range(2)], "htt")
                ksts = Rot([sb(p1a, "kst%d" % i, [128, 4, 256], BF16) for i in range(2)], "kst")
                vsts = Rot([sb(p1a, "vst%d" % i, [128, 2, 512], BF16) for i in range(2)], "vst")
                load_w(5632, 512, wbf[0], "wbf0")
                load_w(6144, 512, wbf[1], "wbf1")
                NT1 = VT // 256
                pbi = 0
                psi = 0
                nst = {}

                xld = {}

                def stage_l(i):
                    xt, xres = xts.next()
                    tr.dma("sp", xt[:], xs[256 * i:256 * i + 256, :].rearrange("(s p) f -> p s f", p=128),
                           writes=[xres], slot=xres)
                    xld[i] = (xt, xres)

                def stage_n(i):
                    if i + 2 < NT1:
                        stage_l(i + 2)
                    xt, xres = xld.pop(i)
                    hb, hbres = hbs.next()
                    ssb, ssres = sss.next()
                    rms_tile(st, xt, xres, 2, hb, hbres, ssb, ssres)
                    nst[i] = (hb, hbres)

                mst = {}
                cnt1 = {"pbi": 0, "psi": 0}

                def stage_mt(i):
                    hb, hbres = nst.pop(i)
                    resident = (256 * i >= VT - HT)
                    if resident:
                        u0 = 256 * i - (VT - HT)
                        hdst = lambda fc, u0=u0: hT[:, fc, u0:u0 + 256]
                        hres = "hTres%d" % i
                    else:
                        htt, hres = hts.next()
                        hdst = lambda fc, htt=htt: htt[:, fc, :]
                    mst[i] = (hdst, hres)
                    for fc in range(8):
                        pb = PB[cnt1["pbi"] % 2]
                        pbres = "pb%d" % (cnt1["pbi"] % 2)
                        cnt1["pbi"] += 1
                        for s in range(2):
                            tr.op("pe", lambda s=s, fc=fc, pb=pb: nc.tensor.transpose(
                                out=pb[:, s * 128:(s + 1) * 128], in_=hb[:, s, fc * 128:(fc + 1) * 128], identity=ident[:]),
                                reads=[hbres], writes=[pbres])
                        evac(hdst(fc), pb[:, 0:256], [pbres], [hres])

                def stage_mm(i):
                    hdst, hres = mst.pop(i)
                    kst, kres = ksts.next()
                    for pr in range(4):
                        ps = PS[cnt1["psi"] % 6]
                        psres = "ps%d" % (cnt1["psi"] % 6)
                        cnt1["psi"] += 1
                        for fc in range(8):
                            tr.op("pe", lambda fc=fc, pr=pr, ps=ps: nc.tensor.matmul(
                                ps[:, 0:256], lhsT=wbf[0][:, fc, pr * 128:(pr + 1) * 128], rhs=hdst(fc),
                                start=(fc == 0), stop=(fc == 7)), reads=["wbf0", hres], writes=[psres])
                        evac(kst[:, pr, :], ps[:, 0:256], [psres], [kres])
                    tr.dma("pool", KTb[:, 256 * i:256 * i + 256].rearrange("(pr p) t -> p pr t", p=128), kst[:],
                           reads=[kres], writes=[], slot=kres)
                    vst, vres = vsts.next()
                    for s in range(2):
                        ps = PS[cnt1["psi"] % 6]
                        psres = "ps%d" % (cnt1["psi"] % 6)
                        cnt1["psi"] += 1
                        for fc in range(8):
                            tr.op("pe", lambda fc=fc, s=s, ps=ps: nc.tensor.matmul(
                                ps[:, :], lhsT=hdst(fc)[:, s * 128:(s + 1) * 128], rhs=wbf[1][:, fc, :],
                                start=(fc == 0), stop=(fc == 7)), reads=["wbf1", hres], writes=[psres])
                        evac(vst[:, s, :], ps[:, :], [psres], [vres])
                    tr.dma("pool", Vb[256 * i:256 * i + 256, :].rearrange("(s p) c -> p s c", p=128), vst[:],
                           reads=[vres], writes=[], slot=vres)

                stage_l(0)
                stage_l(1)
                stage_n(0)
                stage_n(1)
                stage_mt(0)
                for i in range(NT1):
                    if i + 1 < NT1:
                        stage_mt(i + 1)
                    if i + 2 < NT1:
                        stage_n(i + 2)
                    stage_mm(i)
                tr.barrier()

            with contextlib.ExitStack() as p1b:
                fsts = Rot([sb(p1b, "fst%d" % i, [128, 512], BF16) for i in range(4)], "fst")
                gths = Rot([sb(p1b, "gth%d" % i, [128, 8, 512], BF16) for i in range(3)], "gth")
                gst = [None]
                psi = [0]

                blocks = []
                gci = [0]

                def gcopy(out, in_, gres):
                    gci[0] += 1
                    if gci[0] % 2:
                        tr.op("dve", lambda: nc.vector.tensor_copy(out=out, in_=in_), writes=[gres])
                    else:
                        tr.op("act", lambda: nc.scalar.copy(out=out, in_=in_), writes=[gres])

                def fm_block(col0, ncols, tiles, func=None, bias_col0=None, gather=False):
                    def units(wb, wres):
                        us = []
                        if gather:
                            gmap = {}
                            ugs = []
                            for ti, (tsl, N, dstf) in enumerate(tiles):
                                def ug(ti=ti, tsl=tsl, N=N):
                                    gt_, gres = gths.next()
                                    for fc in range(8):
                                        gcopy(gt_[:, fc, 0:N], hT[:, fc, tsl], gres)
                                    gmap[ti] = (gt_, gres)
                                ugs.append(ug)
                            us.append(ugs[0])
                            for ti, (tsl, N, dstf) in enumerate(tiles):
                                if ti + 1 < len(tiles):
                                    us.append(ugs[ti + 1])
                                for cc in range(ncols // 128):
                                    def u(ti=ti, cc=cc, N=N, dstf=dstf):
                                        gt_, gres = gmap[ti]
                                        ps = PS[psi[0] % 6]
                                        psres = "ps%d" % (psi[0] % 6)
                                        psi[0] += 1
                                        for fc in range(8):
                                            tr.op("pe", lambda fc=fc: nc.tensor.matmul(
                                                ps[:, 0:N], lhsT=wb[:, fc, cc * 128:(cc + 1) * 128], rhs=gt_[:, fc, 0:N],
                                                start=(fc == 0), stop=(fc == 7)), reads=[wres, gres], writes=[psres])
                                        fst, fres = fsts.next()
                                        evac(fst[:, 0:N], ps[:, 0:N], [psres], [fres], func=func)
                                        tr.dma("pool" if psi[0] % 2 else "sp", dstf(cc), fst[:, 0:N], reads=[fres], writes=[], slot=fres)
                                    us.append(u)
                            return us
                        for cc in range(ncols // 128):
                            for (tsl, N, dstf) in tiles:
                                def u(cc=cc, tsl=tsl, N=N, dstf=dstf):
                                    ps = PS[psi[0] % 6]
                                    psres = "ps%d" % (psi[0] % 6)
                                    psi[0] += 1
                                    for fc in range(8):
                                        tr.op("pe", lambda fc=fc: nc.tensor.matmul(
                                            ps[:, 0:N], lhsT=wb[:, fc, cc * 128:(cc + 1) * 128], rhs=hT[:, fc, tsl],
                                            start=(fc == 0), stop=(fc == 7)), reads=[wres], writes=[psres])
                                    fst, fres = fsts.next()
                                    b = None if bias_col0 is None else bmg[:, bias_col0 + cc:bias_col0 + cc + 1]
                                    evac(fst[:, 0:N], ps[:, 0:N], [psres], [fres], func=func, bias=b)
                                    tr.dma("pool" if psi[0] % 2 else "sp", dstf(cc), fst[:, 0:N], reads=[fres], writes=[], slot=fres)
                                us.append(u)
                        return us
                    blocks.append((col0, ncols, units))

                def tm_block(col0, ncols, tiles, func=None, gather=False):
                    def units(wb, wres):
                        us = []
                        for (tsl, dst) in tiles:
                            def u(tsl=tsl, dst=dst):
                                if gather:
                                    gt_, gres = gths.next()
                                    for fc in range(8):
                                        gcopy(gt_[:, fc, 0:128], hT[:, fc, tsl], gres)
                                    lt = lambda fc: gt_[:, fc, 0:128]
                                    rds = [wres, gres]
                                else:
                                    lt = lambda fc: hT[:, fc, tsl]
                                    rds = [wres]
                                ps = PS[psi[0] % 6]
                                psres = "ps%d" % (psi[0] % 6)
                                psi[0] += 1
                                for fc in range(8):
                                    tr.op("pe", lambda fc=fc: nc.tensor.matmul(
                                        ps[:, 0:ncols], lhsT=lt(fc), rhs=wb[:, fc, 0:ncols],
                                        start=(fc == 0), stop=(fc == 7)), reads=rds, writes=[psres])
                                fst, fres = fsts.next()
                                evac(fst[:, 0:ncols], ps[:, 0:ncols], [psres], [fres], func=func)
                                tr.dma("pool" if psi[0] % 2 else "sp", dst, fst[:, 0:ncols], reads=[fres], writes=[], slot=fres)
                            us.append(u)
                        return us
                    blocks.append((col0, ncols, units))

                own_fm = lambda dst: [(slice(HALO + 512 * i, HALO + 512 * i + 512), 512,
                                       (lambda cc, i=i: dst[cc * 128:(cc + 1) * 128, 512 * i:512 * i + 512]))
                                      for i in range(8)]
                own_tm = lambda dst, nco: [(slice(HALO + 128 * i, HALO + 128 * i + 128), dst[128 * i:128 * i + 128, 0:nco])
                                           for i in range(32)]
                for g, d in enumerate(DILS):
                    c0 = 1536 * g
                    tiles = []
                    nq = T // d
                    for r in range(d):
                        N = min(512, nq)
                        for i0 in range(0, nq, N):
                            u0 = HALO + r + d * i0
                            tiles.append((slice(u0, u0 + d * (N - 1) + 1, d), N,
                                          (lambda cc, r=r, i0=i0, N=N, g=g, nq=nq:
                                           QTg[g][cc * 128:(cc + 1) * 128, r * nq + i0:r * nq + i0 + N])))
                    fm_block(c0, 512, tiles, gather=(d > 1))
                    tiles = []
                    nk = HT // d
                    i_first = HALO // d - 128
                    for r in range(d):
                        i0 = i_first
                        while i0 < nk:
                            N = min(512, nk - i0)
                            u0 = r + d * i0
                            tiles.append((slice(u0, u0 + d * (N - 1) + 1, d), N,
                                          (lambda cc, r=r, i0=i0, N=N, g=g, nk=nk:
                                           KTg[g][cc * 128:(cc + 1) * 128, r * nk + i0:r * nk + i0 + N])))
                            i0 += N
                    fm_block(c0 + 512, 512, tiles, gather=(d > 1))
                    tiles = []
                    for r in range(d):
                        for kb in range(HALO // d // 128 - 1, nk // 128):
                            u0 = r + d * 128 * kb
                            p0 = r * nk + 128 * kb
                            tiles.append((slice(u0, u0 + d * 127 + 1, d), Vg[g][p0:p0 + 128, :]))
                    tm_block(c0 + 1024, 512, tiles, gather=(d > 1))
                tm_block(4608, 512, own_tm(Ga, 512), func=AF.Silu)
                fm_block(5120, 512, own_fm(QTb))
                tm_block(6656, 512, own_tm(Gb, 512), func=AF.Silu)
                fm_block(7168, 256, own_fm(MQT))
                tm_block(7424, 256, own_tm(Gm, 256), func=AF.Silu)
                for j in range(6):
                    fm_block(7680 + 512 * j, 512,
                             [(sl, N, (lambda cc, f=f, j=j: f(cc + 4 * j))) for (sl, N, f) in own_fm(SG)],
                             func=AF.Sigmoid, bias_col0=4 * j)

                def w_dma(n):
                    col0, ncols, _ = blocks[n]
                    tr.dma("sp", wst[:, :, 0:ncols], w_in[:, col0:col0 + ncols].rearrange("(fc p) c -> p fc c", p=128),
                           writes=["wst"], slot="wst")

                def w_conv(n):
                    col0, ncols, _ = blocks[n]
                    k = n % 2
                    for fc in range(8):
                        conv_scale(wbf[k][:, fc, 0:ncols], wst[:, fc, 0:ncols], nw[:, fc:fc + 1], ["wst"], ["wbf%d" % k])

                w_dma(0)
                w_conv(0)
                for n in range(len(blocks)):
                    if n + 1 < len(blocks):
                        w_dma(n + 1)
                    us = blocks[n][2](wbf[n % 2], "wbf%d" % (n % 2))
                    for ui, u in enumerate(us):
                        if ui == (2 * len(us)) // 3 and n + 1 < len(blocks):
                            w_conv(n + 1)
                        u()
                tr.barrier()

        if "stop1" in dbg:
            tr.finish()
            return nc

        with contextlib.ExitStack() as p2:
            bm = sb(p2, "bm", [128, 3 * 8 * 256], F32)
            tr.dma("sp", bm[:], bm_d, writes=["bm"], slot="ld0")
            qts = Rot([sb(p2, "qt%d" % i, [128, T], BF16) for i in range(2)], "qt")
            kts = Rot([sb(p2, "kt%d" % i, [128, HT], BF16) for i in range(2)], "kt")
            vas = Rot([sb(p2, "va%d" % i, [128, 48, 2, 65], BF16) for i in range(2)], "va")
            efs = Rot([sb(p2, "ef%d" % i, [128, 256], F32) for i in range(3)], "ef")
            ems = Rot([sb(p2, "em%d" % i, [128, 256], BF16) for i in range(4)], "em")
            osbs = Rot([sb(p2, "osb%d" % i, [128, 32, 2, 65], F32) for i in range(2)], "osb")
            for va in vas.tiles:
                tr.op("pool", lambda va=va: nc.gpsimd.memset(va[:, :, :, 64:65], 1.0), writes=["va0", "va1"])
            tr.barrier()
            for g, d in enumerate(DILS):
                nk = HT // d
                nq = T // d
                nkb = nk // 128
                qb0 = nkb - nq // 128
                nqo = nq // 128
                for hp in range(4):
                    qt, qres = qts.next()
                    kt, kres = kts.next()
                    va, vres = vas.next()
                    tr.dma("sp", qt[:], QTg[g][hp * 128:(hp + 1) * 128, :], writes=[qres], slot=qres)
                    tr.dma("sp", kt[:], KTg[g][hp * 128:(hp + 1) * 128, :], writes=[kres], slot=kres)
                    for hh in range(2):
                        for part in range(2):
                            tr.dma("sp", va[:, 24 * part:24 * part + 24, hh, 0:64],
                                   Vg[g][3072 * part:3072 * part + 3072, hp * 128 + 64 * hh:hp * 128 + 64 * hh + 64].rearrange(
                                       "(c p) e -> p c e", p=128),
                                   writes=[vres], slot="%sp%d" % (vres, part % 2))
                    steps = []
                    for r in range(d):
                        osb, ores = osbs.next()
                        for hh in range(2):
                            for kb in range(qb0 - 1, nkb):
                                has_cur = kb >= qb0
                                has_prev = kb + 1 < nkb
                                if not (has_cur or has_prev):
                                    continue
                                steps.append(dict(r=r, hh=hh, kb=kb, has_cur=has_cur, has_prev=has_prev, osb=osb, ores=ores,
                                                  last=(hh == 1 and kb == nkb - 1)))
                    ns = len(steps)

                    def stage_a(i):
                        sp_ = steps[i]
                        r, hh, kb, has_cur, has_prev = sp_["r"], sp_["hh"], sp_["kb"], sp_["has_cur"], sp_["has_prev"]
                        h = 2 * hp + hh
                        rows = slice(64 * hh, 64 * hh + 64)
                        bmo = (g * 8 + h) * 256
                        qlo = (kb if has_cur else kb + 1) - qb0
                        qhi = (kb + 1 if has_prev else kb) - qb0
                        ncol = 128 * (qhi - qlo + 1)
                        qc0 = r * nq + 128 * qlo
                        kc0 = r * nk + 128 * kb
                        ps_s = PS[i % 3]
                        ps_sres = "ps%d" % (i % 3)
                        tr.op("pe", lambda: nc.tensor.matmul(
                            ps_s[:, 0:ncol], lhsT=kt[rows, kc0:kc0 + 128], rhs=qt[rows, qc0:qc0 + ncol],
                            start=True, stop=True), reads=[kres, qres], writes=[ps_sres])
                        ef, efres = efs.next()
                        tr.op("act", lambda: nc.scalar.activation(
                            out=ef[:, 0:ncol], in_=ps_s[:, 0:ncol], func=AF.Exp, scale=SCALE),
                            reads=[ps_sres], writes=[efres])
                        em, emres = ems.next()
                        b0 = bmo if has_cur else bmo + 128
                        if kb < qb0:
                            tr.op("dve", lambda: nc.vector.scalar_tensor_tensor(
                                out=em[:, 0:ncol], in0=ef[:, 0:ncol], scalar=hv[:, 0:1], in1=bm[:, b0:b0 + ncol],
                                op0=ALU.mult, op1=ALU.mult), reads=[efres, "bm"], writes=[emres])
                        else:
                            tr.op("dve", lambda: nc.vector.tensor_tensor(
                                out=em[:, 0:ncol], in0=ef[:, 0:ncol], in1=bm[:, b0:b0 + ncol], op=ALU.mult),
                                reads=[efres, "bm"], writes=[emres])
                        sp_["em"] = (em, emres)

                    def stage_b(i):
                        sp_ = steps[i]
                        r, hh, kb, has_cur, has_prev = sp_["r"], sp_["hh"], sp_["kb"], sp_["has_cur"], sp_["has_prev"]
                        osb, ores = sp_["osb"], sp_["ores"]
                        em, emres = sp_["em"]
                        vch = r * nkb + kb
                        col = 0
                        if has_cur:
                            po = PS[3 + (kb % 2)]
                            pores = "ps%d" % (3 + (kb % 2))
                            tr.op("pe", lambda: nc.tensor.matmul(
                                po[:, 0:65], lhsT=em[:, 0:128], rhs=va[:, vch, hh, :], start=False, stop=True),
                                reads=[emres, vres], writes=[pores])
                            qo = kb - qb0
                            evac(osb[:, qo, hh, :], po[:, 0:65], [pores], [ores], eng="dve" if i % 3 else "act")
                            col = 128
                        if has_prev:
                            po = PS[3 + ((kb + 1) % 2)]
                            pores = "ps%d" % (3 + ((kb + 1) % 2))
                            tr.op("pe", lambda: nc.tensor.matmul(
                                po[:, 0:65], lhsT=em[:, col:col + 128], rhs=va[:, vch, hh, :], start=True, stop=False),
                                reads=[emres, vres], writes=[pores])
                        if sp_["last"]:
                            tr.dma("pool", OD[g][r:T:d, 2 * hp:2 * hp + 2, :].rearrange("(qo a) h e -> a qo h e", a=128),
                                   osb[:, 0:nqo, :, :], reads=[ores], writes=[], slot=ores)

                    stage_a(0)
                    if ns > 1:
                        stage_a(1)
                    for i in range(ns):
                        if i + 2 < ns:
                            stage_a(i + 2)
                        stage_b(i)
            tr.barrier()

        if "stop2" in dbg:
            tr.finish()
            return nc

        with contextlib.ExitStack() as p3:
            kas = Rot([sb(p3, "ka%d" % i, [128, VT], BF16) for i in range(2)], "ka")
            qas = Rot([sb(p3, "qa%d" % i, [128, T], BF16) for i in range(2)], "qa")
            vbs_ = Rot([sb(p3, "vv%d" % i, [128, 128, 65], BF16) for i in range(2)], "vv")
            ab = sb(p3, "ab", [128, 8 * NM], F32)
            ct = sb(p3, "ct", [128, 32], F32)
            pm = sb(p3, "pm", [128, 128], F32)
            cb = sb(p3, "cb", [128, 4 * 512], F32)
            vbt = sb(p3, "vbt", [128, 64], F32)
            ksum = sb(p3, "ksum", [64, 64], F32)
            kmb = sb(p3, "kmb", [64, 64], BF16)
            g1s = Rot([sb(p3, "g1%d" % i, [128, 64], F32) for i in range(2)], "g1")
            t8s = Rot([sb(p3, "t8%d" % i, [128, 16], F32) for i in range(2)], "t8")
            sms = Rot([sb(p3, "sm%d" % i, [128, 64], F32) for i in range(2)], "sm")
            mbs = Rot([sb(p3, "mb%d" % i, [128, 128], BF16) for i in range(2)], "mb")
            ets = Rot([sb(p3, "et%d" % i, [128, 512], BF16) for i in range(3)], "et")
            tmps = Rot([sb(p3, "tmpf%d" % i, [128, 512], F32) for i in range(2)], "tmpf")
            otf = sb(p3, "otf", [65, 512], F32)
            obs = Rot([sb(p3, "obst%d" % i, [128, 4, 65], F32) for i in range(2)], "obst")
            for i, (dst, src) in enumerate([(ab, ab_d), (ct, ct_d), (pm, pm_d), (cb, cb_d), (vbt, vb_d)]):
                tr.dma("sp", dst[:], src, writes=["k%d" % i], slot="ld%d" % (i % 2))
            for i, ka in enumerate(kas.tiles):
                tr.dma("sp", ka[64:128, :], sel_d, writes=["ka%d" % i], slot="ld%d" % (i % 2))
            for i, vv in enumerate(vbs_.tiles):
                tr.op("pool", lambda vv=vv: nc.gpsimd.memset(vv[:, :, 64:65], 1.0), writes=["vv%d" % i])
            for i, mb in enumerate(mbs.tiles):
                tr.op("pool", lambda mb=mb: nc.gpsimd.memset(mb[:, 0:64], 0.0), writes=["mb%d" % i])
            tr.barrier()
            otfs = Rot([otf, sb(p3, "otf1", [65, 512], F32)], "otf")
            bufs = {}

            def loads(h):
                ka, kares = kas.next()
                qa, qares = qas.next()
                vv, vvres = vbs_.next()
                bufs[h] = (ka, kares, qa, qares, vv, vvres)
                for part in range(4):
                    tr.dma("sp", ka[0:64, 4096 * part:4096 * part + 4096],
                           KTb[64 * h:64 * h + 64, 4096 * part:4096 * part + 4096],
                           writes=[kares], slot="%sp%d" % (kares, part % 2))
                tr.dma("sp", qa[0:64, :], QTb[64 * h:64 * h + 64, :], writes=[qares + "q"], slot=qares)
                for part in range(8):
                    tr.dma("sp", vv[:, 16 * part:16 * part + 16, 0:64],
                           Vb[2048 * part:2048 * part + 2048, 64 * h:64 * h + 64].rearrange("(c p) e -> p c e", p=128),
                           writes=[vvres], slot="%sp%d" % (vvres, part % 2))

            def gate_tasks(h):
                ka, kares, qa, qares, vv, vvres = bufs[h]
                tasks = []

                def t_kmean():
                    tr.op("dve", lambda: nc.vector.tensor_reduce(
                        out=ksum[:, :], in_=ka[0:64, :].rearrange("p (n k) -> p n k", k=256), axis=AX.X, op=ALU.add),
                        reads=[kares], writes=["ksum"])
                    tr.op("dve", lambda: nc.vector.tensor_copy(out=kmb[:, :], in_=ksum[:, :]), reads=["ksum"], writes=["kmb"])
                tasks.append(t_kmean)
                st = {}
                for qi in range(32):
                    def ta(qi=qi):
                        b_own = (VT - T) // 256 + qi // 2
                        pg = PS[5]
                        tr.op("pe", lambda: nc.tensor.matmul(
                            pg[:, 0:64], lhsT=qa[0:64, 128 * qi:128 * qi + 128], rhs=kmb[:, :], start=True, stop=True),
                            reads=[qares + "q", "kmb"], writes=["ps5"])
                        g1, g1res = g1s.next()
                        tr.op("dve", lambda: nc.vector.tensor_tensor(
                            out=g1[:], in0=pg[:, 0:64], in1=pm[:, 64 - b_own:128 - b_own], op=ALU.add),
                            reads=["ps5"], writes=[g1res])
                        tr.op("dve", lambda: nc.vector.tensor_tensor(out=g1[:], in0=g1[:], in1=vbt[:], op=ALU.add),
                              reads=[g1res], writes=[g1res])
                        t8, t8res = t8s.next()
                        tr.op("dve", lambda: nc.vector.max(out=t8[:, 0:8], in_=g1[:]), reads=[g1res], writes=[t8res])
                        tr.op("dve", lambda: nc.vector.tensor_scalar_max(out=t8[:, 8:9], in0=t8[:, 2:3], scalar1=-1e29),
                              reads=[t8res], writes=[t8res])
                        sm, smres = sms.next()
                        tr.op("dve", lambda: nc.vector.tensor_scalar(
                            out=sm[:], in0=g1[:], scalar1=t8[:, 8:9], scalar2=None, op0=ALU.is_ge),
                            reads=[g1res, t8res], writes=[smres])
                        tr.op("dve", lambda: nc.vector.memset(sm[:, b_own:b_own + 1], 1.0), reads=[smres], writes=[smres])
                        mb, mbres = mbs.next()
                        cti = 4 * h + qi % 4
                        tr.op("dve", lambda: nc.vector.tensor_scalar(
                            out=mb[:, 64:128], in0=sm[:], scalar1=ct[:, cti:cti + 1], scalar2=MBNEG, op0=ALU.mult, op1=ALU.add),
                            reads=[smres], writes=[mbres])
                        st[qi] = (mb, mbres)

                    def tb(qi=qi):
                        mb, mbres = st[qi]
                        pb = PB[qi % 2]
                        pbres = "pb%d" % (qi % 2)
                        tr.op("pe", lambda: nc.tensor.transpose(out=pb[:, 0:128], in_=mb[:, :], identity=ident[:]),
                              reads=[mbres], writes=[pbres])
                        tr.op("dve", lambda: nc.vector.tensor_copy(
                            out=qa[64:128, 128 * qi:128 * qi + 128], in_=pb[64:128, 0:128]),
                            reads=[pbres], writes=[qares + "m%d" % (qi // 4)])
                    tasks.append(ta)
                    tasks.append(tb)
                order = [tasks[0], tasks[1]]
                for qi in range(32):
                    if qi + 1 < 32:
                        order.append(tasks[1 + 2 * (qi + 1)])
                    order.append(tasks[2 + 2 * qi])
                return order

            def main_loop(h, pending):
                ka, kares, qa, qares, vv, vvres = bufs[h]
                items = []
                firsts = {}
                for qt_ in range(8):
                    nch = (VT - T) // 128 + 4 * (qt_ + 1)
                    c_lo = 0
                    if SKIP_FAR:
                        slope = 2.0 ** (-(h + 1))
                        dmin = SKIP_NATS / slope
                        t0q = (VT - T) + 512 * qt_
                        c_lo = max(0, int(np.floor((t0q - 127 - dmin) / 128.0)) + 1)
                    firsts[qt_] = c_lo
                    for c in range(c_lo, nch):
                        items.append((qt_, c, nch))
                n = len(items)
                ets_ = {}
                deferred = {}
                every = max(1, (n - 40) // max(1, len(pending)))

                def stage_a(i):
                    qt_, c, nch = items[i]
                    mi = c - 4 * qt_ + 28
                    ps_s = PS[i % 3]
                    ps_sres = "ps%d" % (i % 3)
                    tr.op("pe", lambda: nc.tensor.matmul(
                        ps_s[:, :], lhsT=ka[:, 128 * c:128 * c + 128], rhs=qa[:, 512 * qt_:512 * qt_ + 512],
                        start=True, stop=True), reads=[kares, qares + "q", qares + "m%d" % qt_], writes=[ps_sres])
                    et, etres = ets.next()
                    ets_[i] = (et, etres)
                    bias = ab[:, h * NM + mi:h * NM + mi + 1]
                    dc = c - (nch - 4)
                    if dc < 0:
                        tr.op("act", lambda: nc.scalar.activation(
                            out=et[:], in_=ps_s[:, :], func=AF.Exp, bias=bias, scale=SCALE),
                            reads=[ps_sres], writes=[etres])
                    else:
                        tmpf, tres = tmps.next()
                        tr.op("dve", lambda: nc.vector.tensor_tensor(
                            out=tmpf[:], in0=ps_s[:, :], in1=cb[:, 512 * dc:512 * dc + 512], op=ALU.add),
                            reads=[ps_sres], writes=[tres])
                        tr.op("act", lambda: nc.scalar.activation(
                            out=et[:], in_=tmpf[:], func=AF.Exp, bias=bias, scale=SCALE),
                            reads=[tres], writes=[etres])

                def stage_b(i):
                    qt_, c, nch = items[i]
                    et, etres = ets_.pop(i)
                    po = PS[3 + qt_ % 2]
                    pores = "ps%d" % (3 + qt_ % 2)
                    tr.op("pe", lambda: nc.tensor.matmul(
                        po[0:65, :], lhsT=vv[:, c, :], rhs=et[:], start=(c == firsts[qt_]), stop=(c == nch - 1)),
                        reads=[etres, vvres], writes=[pores])
                    if c == nch - 1:
                        of, ofres = otfs.next()
                        tr.op("dve", lambda: nc.vector.tensor_copy(out=of[:, :], in_=po[0:65, :]), reads=[pores], writes=[ofres])

                        def fin(qt_=qt_, of=of, ofres=ofres):
                            px = PS[5]
                            for s in range(4):
                                tr.op("pe", lambda s=s: nc.tensor.transpose(
                                    out=px[:, 65 * s:65 * s + 65], in_=of[0:65, 128 * s:128 * s + 128],
                                    identity=identf[0:65, 0:65]), reads=[ofres], writes=["ps5"])
                            obst, obres = obs.next()
                            tr.op("dve", lambda: nc.vector.tensor_copy(
                                out=obst[:].rearrange("p s e -> p (s e)"), in_=px[:, 0:260]), reads=["ps5"], writes=[obres])
                            tr.dma("pool", OB[512 * qt_:512 * qt_ + 512, h, :].rearrange("(s p) e -> p s e", p=128), obst[:],
                                   reads=[obres], writes=[], slot=obres)
                        deferred[min(n - 1, i + 4)] = fin

                stage_a(0)
                stage_a(1)
                for i in range(n):
                    if i + 2 < n:
                        stage_a(i + 2)
                    stage_b(i)
                    if i in deferred:
                        deferred.pop(i)()
                    if pending and i >= 8 and (i - 8) % every == 0:
                        pending.pop(0)()
                for k in sorted(deferred):
                    deferred[k]()
                while pending:
                    pending.pop(0)()

            horder = [7, 6, 5, 4, 3, 2, 1, 0]
            loads(horder[0])
            for t in gate_tasks(horder[0]):
                t()
            for hi, h in enumerate(horder):
                pending = []
                if hi + 1 < 8:
                    loads(horder[hi + 1])
                    pending = gate_tasks(horder[hi + 1])
                main_loop(h, pending)
            tr.barrier()

        if "stop3" in dbg:
            tr.finish()
            return nc

        with contextlib.ExitStack() as p4:
            wst = sb(p4, "wst4", [128, 8, 512], F32)
            wkv = sb(p4, "wkv", [128, 8, 512], BF16)
            mx = sb(p4, "mx", [128, 2, D], F32)
            mhb = sb(p4, "mhb", [128, 2, D], BF16)
            junk = sb(p4, "junk4", [128, D], BF16)
            mss = sb(p4, "mss", [128, 6], F32)
            memT = sb(p4, "memT", [128, 8, 256], BF16)
            mkT = sb(p4, "mkT", [128, 2, 256], BF16)
            mva = sb(p4, "mva", [128, 2, 4, 65], BF16)
            mq = sb(p4, "mq", [128, 2, T], BF16)
            ems = Rot([sb(p4, "em4%d" % i, [128, 2, 512], BF16) for i in range(2)], "em4")
            rds = Rot([sb(p4, "rd4%d" % i, [128, 4], F32) for i in range(2)], "rd4")
            oms = Rot([sb(p4, "om4%d" % i, [128, 4, 64], F32) for i in range(2)], "om4")
            tr.dma("sp", wst[:], w_kv.rearrange("(fc p) c -> p fc c", p=128), writes=["wst4"], slot="ld0")
            tr.dma("sp", mx[:], memb.rearrange("(s p) f -> p s f", p=128), writes=["mx"], slot="ld1")
            tr.dma("sp", mq[:], MQT.rearrange("(pr p) t -> p pr t", p=128), writes=["mq"], slot="ld0")
            tr.op("pool", lambda: nc.gpsimd.memset(mva[:, :, :, 64:65], 1.0), writes=["mva1"])
            for fc in range(8):
                conv_scale(wkv[:, fc, :], wst[:, fc, :], mnw[:, fc:fc + 1], ["wst4"], ["wkv"])
            rms_tile({"junk": junk}, mx, "mx", 2, mhb, "mhb", mss, "mss")
            for fc in range(8):
                pb = PB[fc % 2]
                pbres = "pb%d" % (fc % 2)
                for s in range(2):
                    tr.op("pe", lambda s=s, fc=fc, pb=pb: nc.tensor.transpose(
                        out=pb[:, s * 128:(s + 1) * 128], in_=mhb[:, s, fc * 128:(fc + 1) * 128], identity=ident[:]),
                        reads=["mhb"], writes=[pbres])
                evac(memT[:, fc, :], pb[:, 0:256], [pbres], ["memT"])
            for pr in range(2):
                ps = PS[pr]
                for fc in range(8):
                    tr.op("pe", lambda fc=fc, pr=pr, ps=ps: nc.tensor.matmul(
                        ps[:, 0:256], lhsT=wkv[:, fc, pr * 128:(pr + 1) * 128], rhs=memT[:, fc, :],
                        start=(fc == 0), stop=(fc == 7)), reads=["wkv", "memT"], writes=["ps%d" % pr])
                evac(mkT[:, pr, :], ps[:, 0:256], ["ps%d" % pr], ["mkT"])
            for s in range(2):
                ps = PS[2 + s]
                for fc in range(8):
                    tr.op("pe", lambda fc=fc, s=s, ps=ps: nc.tensor.matmul(
                        ps[:, 0:256], lhsT=memT[:, fc, s * 128:(s + 1) * 128], rhs=wkv[:, fc, 256:512],
                        start=(fc == 0), stop=(fc == 7)), reads=["wkv", "memT"], writes=["ps%d" % (2 + s)])
                evac(mva[:, s, :, 0:64], ps[:, 0:256].rearrange("p (h e) -> p h e", e=64), ["ps%d" % (2 + s)], ["mva"])
            it = 0
            for h in range(4):
                pr = h // 2
                rows = slice(64 * (h % 2), 64 * (h % 2) + 64)
                for qt_ in range(8):
                    em, emres = ems.next()
                    for mc in range(2):
                        ps_s = PS[mc]
                        tr.op("pe", lambda ps_s=ps_s, mc=mc, qt_=qt_, pr=pr, rows=rows: nc.tensor.matmul(
                            ps_s[:, :], lhsT=mkT[rows, pr, 128 * mc:128 * mc + 128], rhs=mq[rows, pr, 512 * qt_:512 * qt_ + 512],
                            start=True, stop=True), reads=["mkT", "mq"], writes=["ps%d" % mc])
                        tr.op("act", lambda em=em, ps_s=ps_s, mc=mc: nc.scalar.activation(
                            out=em[:, mc, :], in_=ps_s[:, :], func=AF.Exp, scale=SCALE), reads=["ps%d" % mc], writes=[emres])
                    po = PS[2 + it % 2]
                    pores = "ps%d" % (2 + it % 2)
                    it += 1
                    for s in range(4):
                        for mc in range(2):
                            tr.op("pe", lambda po=po, s=s, mc=mc, em=em, h=h: nc.tensor.matmul(
                                po[:, 65 * s:65 * s + 65], lhsT=em[:, mc, 128 * s:128 * s + 128], rhs=mva[:, mc, h, :],
                                start=(mc == 0), stop=(mc == 1)), reads=[emres, "mva", "mva1"], writes=[pores])
                    rd, rdres = rds.next()
                    pov = po[:, 0:260].rearrange("p (s e) -> p s e", e=65)
                    tr.op("dve", lambda rd=rd, pov=pov: nc.vector.reciprocal(out=rd[:], in_=pov[:, :, 64]),
                          reads=[pores], writes=[rdres])
                    om, omres = oms.next()
                    tr.op("dve", lambda om=om, rd=rd, pov=pov: nc.vector.tensor_tensor(
                        out=om[:], in0=pov[:, :, 0:64], in1=rd[:].unsqueeze(2).to_broadcast([128, 4, 64]), op=ALU.mult),
                        reads=[pores, rdres], writes=[omres])
                    tr.dma("pool", OM[512 * qt_:512 * qt_ + 512, 64 * h:64 * h + 64].rearrange("(s p) e -> p s e", p=128),
                           om[:], reads=[omres], writes=[], slot=omres)
            tr.barrier()

        if "stop4" in dbg:
            tr.finish()
            return nc

        with contextlib.ExitStack() as p5:
            wst = sb(p5, "wst5", [128, 2, D], F32)
            wbr = sb(p5, "wbr", [128, 10, D], BF16)
            wout = sb(p5, "wout", [128, 8, D], BF16)
            fnw = sb(p5, "fnw", [128, D], F32)
            tr.dma("sp", fnw[:], fnw_d, writes=["fnw"], slot="ld1")
            for (src, nfc, f0, dstw) in ((w_ba, 4, 0, wbr), (w_bb, 4, 4, wbr), (w_bm, 2, 8, wbr), (w_o, 8, 0, wout)):
                for f2 in range(0, nfc, 2):
                    tr.dma("sp", wst[:], src[128 * f2:128 * f2 + 256, :].rearrange("(fc p) c -> p fc c", p=128),
                           writes=["wst5"], slot="ld0")
                    for k in range(2):
                        conv_scale(dstw[:, f0 + f2 + k, :], wst[:, k, :], None, ["wst5"], ["wbr"])
            ods = Rot([sb(p5, "od%d" % i, [128, 3, 8, 65], F32) for i in range(2)], "od")
            obl = Rot([sb(p5, "obl%d" % i, [128, 8, 65], F32) for i in range(2)], "obl")
            oml = Rot([sb(p5, "oml%d" % i, [128, 256], F32) for i in range(2)], "oml")
            gts = Rot([sb(p5, "gt%d" % i, [128, 1280], BF16) for i in range(2)], "gt")
            rdn = Rot([sb(p5, "rdn%d" % i, [128, 16], F32) for i in range(2)], "rdn")
            oas = Rot([sb(p5, "oa%d" % i, [128, 1280], F32) for i in range(2)], "oa")
            ogs = Rot([sb(p5, "og%d" % i, [128, 1280], BF16) for i in range(2)], "og")
            ogT = Rot([sb(p5, "ogT%d" % i, [128, 10, 512], BF16) for i in range(2)], "ogT")
            sgs = Rot([sb(p5, "sg%d" % i, [128, 24, 512], BF16) for i in range(2)], "sg")
            m1s = Rot([sb(p5, "m1%d" % i, [128, 512], F32) for i in range(2)], "m1")
            m2s = Rot([sb(p5, "m2%d" % i, [128, 512], F32) for i in range(2)], "m2")
            m3s = Rot([sb(p5, "m3%d" % i, [128, 512], F32) for i in range(2)], "m3")
            mTs = Rot([sb(p5, "mT%d" % i, [128, 8, 512], BF16) for i in range(2)], "mT")
            xrs = Rot([sb(p5, "xr%d" % i, [128, D], F32) for i in range(2)], "xr")
            ys = Rot([sb(p5, "y%d" % i, [128, D], F32) for i in range(2)], "y")
            junk = sb(p5, "junk5", [128, D], BF16)
            sss = Rot([sb(p5, "ss5%d" % i, [128, 4], F32) for i in range(2)], "ss5")
            pbi = 0
            p5st = {}

            def stage_a5(tt):
                nonlocal pbi
                ogt, ogtres = ogT.next()
                sg, sgres = sgs.next()
                tr.dma("sp", sg[:], SG[:, 512 * tt:512 * tt + 512].rearrange("(c p) t -> p c t", p=128), writes=[sgres], slot=sgres)
                for s in range(4):
                    tok0 = 512 * tt + 128 * s
                    od, odres = ods.next()
                    for g in range(3):
                        tr.dma("sp", od[:, g, :, :], OD[g][tok0:tok0 + 128, :, :], writes=[odres + "g%d" % g],
                               slot="%sg%d" % (odres, g))
                    ob, obres = obl.next()
                    tr.dma("sp", ob[:], OB[tok0:tok0 + 128, :, :], writes=[obres], slot=obres)
                    oml_, omres = oml.next()
                    tr.dma("sp", oml_[:], OM[tok0:tok0 + 128, :], writes=[omres], slot=omres)
                    gt, gtres = gts.next()
                    tr.dma("sp", gt[:, 0:512], Ga[tok0:tok0 + 128, :], writes=[gtres + "a"], slot=gtres + "a")
                    tr.dma("sp", gt[:, 512:1024], Gb[tok0:tok0 + 128, :], writes=[gtres + "b"], slot=gtres + "b")
                    tr.dma("sp", gt[:, 1024:1280], Gm[tok0:tok0 + 128, :], writes=[gtres + "m"], slot=gtres + "m")
                    tr.op("dve", lambda od=od: nc.vector.tensor_tensor(out=od[:, 0], in0=od[:, 0], in1=od[:, 1], op=ALU.add),
                          reads=[odres + "g0", odres + "g1"], writes=[odres + "g0"])
                    tr.op("pool", lambda od=od: nc.gpsimd.tensor_tensor(out=od[:, 0], in0=od[:, 0], in1=od[:, 2], op=ALU.add),
                          reads=[odres + "g0", odres + "g2"], writes=[odres + "g0"])
                    rd, rdres = rdn.next()
                    tr.op("dve", lambda rd=rd, od=od: nc.vector.reciprocal(out=rd[:, 0:8], in_=od[:, 0, :, 64]),
                          reads=[odres + "g0"], writes=[rdres])
                    tr.op("dve", lambda rd=rd, ob=ob: nc.vector.reciprocal(out=rd[:, 8:16], in_=ob[:, :, 64]),
                          reads=[obres, rdres], writes=[rdres])
                    oa, oares = oas.next()
                    tr.op("dve", lambda oa=oa, od=od, rd=rd: nc.vector.tensor_tensor(
                        out=oa[:, 0:512].rearrange("p (h e) -> p h e", e=64), in0=od[:, 0, :, 0:64],
                        in1=rd[:, 0:8].unsqueeze(2).to_broadcast([128, 8, 64]), op=ALU.mult),
                        reads=[odres + "g0", rdres], writes=[oares])
                    tr.op("pool", lambda oa=oa, ob=ob, rd=rd: nc.gpsimd.tensor_tensor(
                        out=oa[:, 512:1024].rearrange("p (h e) -> p h e", e=64), in0=ob[:, :, 0:64],
                        in1=rd[:, 8:16].unsqueeze(2).to_broadcast([128, 8, 64]), op=ALU.mult),
                        reads=[obres, rdres, oares], writes=[oares])
                    og, ogres = ogs.next()
                    tr.op("dve", lambda og=og, oa=oa, gt=gt: nc.vector.tensor_tensor(
                        out=og[:, 0:1024], in0=oa[:, 0:1024], in1=gt[:, 0:1024], op=ALU.mult),
                        reads=[oares, gtres + "a", gtres + "b"], writes=[ogres])
                    tr.op("pool", lambda og=og, oml_=oml_, gt=gt: nc.gpsimd.tensor_tensor(
                        out=og[:, 1024:1280], in0=oml_[:], in1=gt[:, 1024:1280], op=ALU.mult),
                        reads=[omres, gtres + "m", ogres], writes=[ogres])
                    for fc2 in range(0, 10, 2):
                        pb = PB[pbi % 2]
                        pbres = "pb%d" % (pbi % 2)
                        pbi += 1
                        for k in range(2):
                            fc = fc2 + k
                            tr.op("pe", lambda pb=pb, og=og, fc=fc, k=k: nc.tensor.transpose(
                                out=pb[:, 128 * k:128 * k + 128], in_=og[:, 128 * fc:128 * fc + 128], identity=ident[:]),
                                reads=[ogres], writes=[pbres])
                        evac(ogt[:, fc2:fc2 + 2, 128 * s:128 * s + 128], pb[:, 0:256].rearrange("p (k t) -> p k t", t=128),
                             [pbres], [ogtres])
                p5st[tt] = (ogt, ogtres, sg, sgres)

            def stage_b5(tt):
                ogt, ogtres, sg, sgres = p5st.pop(tt)
                mT, mTres = mTs.next()
                for cc in range(8):
                    pa, pbk, pm_ = PS[0], PS[1], PS[2]
                    for (ps, pres, f0, nf) in ((pa, "ps0", 0, 4), (pbk, "ps1", 4, 4), (pm_, "ps2", 8, 2)):
                        for k in range(nf):
                            tr.op("pe", lambda ps=ps, f0=f0, k=k, nf=nf, cc=cc: nc.tensor.matmul(
                                ps[:, :], lhsT=wbr[:, f0 + k, 128 * cc:128 * cc + 128], rhs=ogt[:, f0 + k, :],
                                start=(k == 0), stop=(k == nf - 1)), reads=[ogtres, "wbr"], writes=[pres])
                    m1, m1res = m1s.next()
                    m2, m2res = m2s.next()
                    m3, m3res = m3s.next()
                    tr.op("dve", lambda m1=m1, cc=cc: nc.vector.tensor_tensor(out=m1[:], in0=pa[:, :], in1=sg[:, cc, :], op=ALU.mult),
                          reads=["ps0", sgres], writes=[m1res])
                    tr.op("dve", lambda m2=m2, cc=cc: nc.vector.tensor_tensor(out=m2[:], in0=pbk[:, :], in1=sg[:, 8 + cc, :], op=ALU.mult),
                          reads=["ps1", sgres], writes=[m2res])
                    tr.op("dve", lambda m3=m3, cc=cc: nc.vector.tensor_tensor(out=m3[:], in0=pm_[:, :], in1=sg[:, 16 + cc, :], op=ALU.mult),
                          reads=["ps2", sgres], writes=[m3res])
                    tr.op("pool", lambda m1=m1, m2=m2: nc.gpsimd.tensor_tensor(out=m1[:], in0=m1[:], in1=m2[:], op=ALU.add),
                          reads=[m1res, m2res], writes=[m1res])
                    tr.op("pool", lambda m1=m1, m3=m3, mT=mT, cc=cc: nc.gpsimd.tensor_tensor(
                        out=mT[:, cc, :], in0=m1[:], in1=m3[:], op=ALU.add), reads=[m1res, m3res], writes=[mTres])
                for s in range(4):
                    tok0 = 512 * tt + 128 * s
                    xr, xrres = xrs.next()
                    tr.dma("sp", xr[:], xs[VT - T + tok0:VT - T + tok0 + 128, :], writes=[xrres], slot=xrres)
                    y, yres = ys.next()
                    for half in range(2):
                        ps = PS[3 + half]
                        pres = "ps%d" % (3 + half)
                        for fc in range(8):
                            tr.op("pe", lambda ps=ps, fc=fc, s=s, half=half: nc.tensor.matmul(
                                ps[:, :], lhsT=mT[:, fc, 128 * s:128 * s + 128], rhs=wout[:, fc, 512 * half:512 * half + 512],
                                start=(fc == 0), stop=(fc == 7)), reads=[mTres, "wout"], writes=[pres])
                        tr.op("dve", lambda ps=ps, y=y, xr=xr, half=half: nc.vector.tensor_tensor(
                            out=y[:, 512 * half:512 * half + 512], in0=ps[:, :], in1=xr[:, 512 * half:512 * half + 512], op=ALU.add),
                            reads=[pres, xrres], writes=[yres])
                    ssb, ssres = sss.next()
                    tr.op("pool", lambda ssb=ssb: nc.gpsimd.memset(ssb[:], 0.0), writes=[ssres])
                    tr.op("act", lambda y=y, ssb=ssb: nc.scalar.activation(out=junk[:], in_=y[:], func=AF.Square, accum_out=ssb[:, 0:1]),
                          reads=[yres, ssres], writes=["junk5", ssres])
                    tr.op("act", lambda ssb=ssb: nc.scalar.activation(out=ssb[:, 1:2], in_=ssb[:, 0:1], func=AF.Sqrt, bias=EPS, scale=1.0 / D),
                          reads=[ssres], writes=[ssres])
                    tr.op("dve", lambda ssb=ssb: nc.vector.reciprocal(out=ssb[:, 2:3], in_=ssb[:, 1:2]), reads=[ssres], writes=[ssres])
                    tr.op("dve", lambda y=y, ssb=ssb: nc.vector.scalar_tensor_tensor(
                        out=y[:], in0=y[:], scalar=ssb[:, 2:3], in1=fnw[:], op0=ALU.mult, op1=ALU.mult),
                        reads=[yres, ssres, "fnw"], writes=[yres])
                    tr.dma("pool", out_d[tok0:tok0 + 128, :], y[:], reads=[yres], writes=[], slot=yres)

            stage_a5(0)
            for tt in range(8):
                if tt + 1 < 8:
                    stage_a5(tt + 1)
                stage_b5(tt)
        tr.finish()
    build_program.stats = (dict(tr.cnt), tr.nsem, tr.nwait, tr.ndma)
    return nc


def _const_tables():
    bf = ml_dtypes.bfloat16
    slopes = (2.0 ** (-8.0 * np.arange(1, 17) / 16)).astype(np.float32)
    sa = slopes[0::2].astype(np.float64)
    sbm = slopes[1::2].astype(np.float64)
    k = np.arange(128)[:, None].astype(np.float64)
    a = np.arange(128)[None, :].astype(np.float64)
    bm = np.zeros((128, 3, 8, 256), np.float32)
    for g, d in enumerate(DILS):
        for h in range(8):
            cur = np.where(k <= a, np.exp(-sa[h] * d * (a - k)), 0.0)
            prev = np.where(k >= a, np.exp(-sa[h] * d * (a + 128 - k)), 0.0)
            bm[:, g, h, 0:128] = cur
            bm[:, g, h, 128:256] = prev
    sel = (np.arange(VT)[None, :] // 256 == np.arange(64)[:, None]).astype(np.float32).astype(bf)
    p = np.arange(128)[:, None].astype(np.float64)
    m = (np.arange(NM)[None, :] - 124).astype(np.float64)
    ab = np.zeros((128, 8, NM), np.float32)
    for h in range(8):
        ab[:, h, :] = sbm[h] * (p + 128 * m)
    ct = np.zeros((128, 8, 4), np.float32)
    for h in range(8):
        for s in range(4):
            ct[:, h, s] = -MBNEG - 8.0 * sbm[h] * (p[:, 0] + 128 * s)
    pm = np.concatenate([np.zeros((128, 64), np.float32), np.full((128, 64), NEG, np.float32)], axis=1)
    cb = np.zeros((128, 4, 512), np.float32)
    tl = np.arange(512)[None, :]
    for c in range(4):
        kp = 128 * c + np.arange(128)[:, None]
        same = (tl // 256) == (c // 2)
        cb[:, c, :] = np.where(same & (kp > tl), -8e5, 0.0)
    return dict(
        ident=np.eye(128, dtype=np.float32).astype(bf), identf=np.eye(128, dtype=np.float32),
        bm=bm.reshape(128, -1), sel=sel, ab=ab.reshape(128, -1), ct=ct.reshape(128, -1), pm=pm,
        cb=cb.reshape(128, -1))


def make_in_maps(x, mem, norm_w, mem_norm_w, w_in, b_merge, w_mem_kv, w_branch_a, w_branch_b, w_branch_m, w_out,
                 final_norm_w):
    f = lambda a: np.ascontiguousarray(np.asarray(a, dtype=np.float32))
    consts = _const_tables()
    shared = dict(
        w_in=f(w_in[0]), w_mem_kv=f(w_mem_kv[0]), w_branch_a=f(w_branch_a[0]), w_branch_b=f(w_branch_b[0]),
        w_branch_m=f(w_branch_m[0]), w_out=f(w_out[0]),
        nw=f(np.asarray(norm_w[0]).reshape(8, 128).T), mnw=f(np.asarray(mem_norm_w[0]).reshape(8, 128).T),
        bmg=f(np.asarray(b_merge[0]).reshape(24, 128).T),
        fnw=f(np.broadcast_to(np.asarray(final_norm_w)[None, :], (128, D))), **consts)
    x = np.asarray(x)
    mem = np.asarray(mem)
    in_maps = []
    for c in range(8):
        b, j = c // 4, c % 4
        xsv = np.zeros((VT, D), np.float32)
        n = (j + 1) * T
        xsv[VT - n:] = x[b, :n]
        vbv = np.where(np.arange(64) >= (3 - j) * 16, 0.0, NEG).astype(np.float32)
        m = dict(shared)
        m.update(xs=xsv, memb=f(mem[b]), vb=f(np.broadcast_to(vbv[None, :], (128, 64))),
                 hv=np.full((128, 1), 1.0 if j > 0 else 0.0, np.float32))
        in_maps.append(m)
    return in_maps


def kernel(**inputs):
    in_maps = make_in_maps(**inputs)
    nc = build_program()
    res = run_bass_kernel_spmd(nc, in_maps, core_ids=list(range(8)))
    out = np.zeros((2, 4 * T, D), np.float32)
    for c in range(8):
        b, j = c // 4, c % 4
        out[b, j * T:(j + 1) * T] = res.results[c]["out"]
    return out
```

```python
import contextlib
import numpy as np
import ml_dtypes
import concourse.bass as bass
import concourse.mybir as mybir
from concourse.bass_utils import run_bass_kernel_spmd

F32 = mybir.dt.float32
BF16 = mybir.dt.bfloat16
AF = mybir.ActivationFunctionType
ALU = mybir.AluOpType
AX = mybir.AxisListType

D = 1024
T = 4096
VT = 16384
HALO = 2048
HT = HALO + T
DIN = 10752
DILS = (1, 4, 16)
SCALE = 0.125
EPS = 1e-6
NEG = -1e30
MBNEG = -30000.0
NM = 160
SKIP_FAR = True
SKIP_NATS = 164.0


class Tr:
    EPOCH = 16000
    DEPOCH = 1000

    def __init__(self, nc):
        self.nc = nc
        self.eng = {"pe": nc.tensor, "act": nc.scalar, "dve": nc.vector, "pool": nc.gpsimd, "sp": nc.sync}
        self.cnt = {e: 0 for e in ("pe", "act", "dve", "pool")}
        self.esem = {e: [] for e in self.cnt}
        self.slot = {}
        self.lastw = {}
        self.readers = {}
        self.waited = {e: {} for e in self.eng}
        self.nsem = 0
        self.nwait = 0
        self.ndma = 0
        self.sems = {}
        self.free_slots = []

    def _newsem(self, name):
        s = self.nc.alloc_semaphore(name)
        self.nsem += 1
        self.sems[id(s)] = s
        return s

    @staticmethod
    def _add(deps, ev):
        k = id(ev[0])
        if k not in deps or deps[k][1] < ev[1]:
            deps[k] = ev

    def _deps(self, reads, writes):
        deps = {}
        for r in reads:
            if r in self.lastw:
                self._add(deps, self.lastw[r])
        for w in writes:
            if w in self.lastw:
                self._add(deps, self.lastw[w])
            for ev in self.readers.get(w, {}).values():
                self._add(deps, ev)
        return deps

    def _emit_waits(self, eng, deps):
        e = self.eng[eng]
        wd = self.waited[eng]
        for k, (sem, val, peng) in deps.items():
            if peng == eng and eng == "pe":
                continue
            if wd.get(k, 0) >= val:
                continue
            e.wait_ge(sem, val)
            self.nwait += 1
            wd[k] = val

    def _record(self, ev, reads, writes):
        for w in writes:
            self.lastw[w] = ev
            self.readers[w] = {}
        for r in reads:
            if r in writes:
                continue
            self._add(self.readers.setdefault(r, {}), ev)

    def op(self, eng, fn, reads=(), writes=()):
        deps = self._deps(reads, writes)
        self._emit_waits(eng, deps)
        ins = fn()
        n = self.cnt[eng]
        ep = n // self.EPOCH
        if ep >= len(self.esem[eng]):
            self.esem[eng].append(self._newsem("e_%s_%d" % (eng, ep)))
        sem = self.esem[eng][ep]
        val = n % self.EPOCH + 1
        ins.then_inc(sem, 1)
        self.cnt[eng] = n + 1
        ev = (sem, val, eng)
        self._record(ev, reads, writes)
        return ev

    def dma(self, q, out, in_, reads=(), writes=(), slot=None):
        deps = self._deps(reads, writes)
        st = self.slot.get(slot)
        if st is None and self.free_slots:
            st = self.free_slots.pop()
            self.slot[slot] = st
        if st is None or st[1] >= self.DEPOCH:
            if st is not None:
                self._add(deps, (st[0], 16 * st[1], "dma"))
            st = [self._newsem("d_%s" % slot), 0]
            self.slot[slot] = st
        elif st[1] > 0:
            self._add(deps, (st[0], 16 * st[1], "dma"))
        self._emit_waits(q, deps)
        ins = self.eng[q].dma_start(out=out, in_=in_)
        st[1] += 1
        self.ndma += 1
        ins.then_inc(st[0], 16)
        ev = (st[0], 16 * st[1], "dma")
        self._record(ev, reads, writes)
        return ev

    def _all_events(self):
        deps = {}
        for e, n in self.cnt.items():
            if n > 0:
                ep = (n - 1) // self.EPOCH
                self._add(deps, (self.esem[e][ep], (n - 1) % self.EPOCH + 1, "x"))
        for st in list(self.slot.values()) + self.free_slots:
            if st[1] > 0:
                self._add(deps, (st[0], 16 * st[1], "dma"))
        return deps

    def barrier(self):
        deps = self._all_events()
        for e in self.eng:
            self._emit_waits(e, dict(deps))
        self.lastw = {}
        self.readers = {}
        self.free_slots.extend(self.slot.values())
        self.slot = {}

    def finish(self):
        self._emit_waits("sp", self._all_events())


class Rot:
    def __init__(self, tiles, name):
        self.tiles = tiles
        self.name = name
        self.i = 0

    def next(self):
        k = self.i % len(self.tiles)
        self.i += 1
        return self.tiles[k], "%s%d" % (self.name, k)


def build_program(dbg=()):
    nc = bass.Bass("TRN2", target_bir_lowering=False)
    tr = Tr(nc)

    def din(name, shape, dt=F32):
        return nc.dram_tensor(name, list(shape), dt, kind="ExternalInput").ap()

    def dscr(name, shape, dt):
        kind = "ExternalOutput" if name in dbg else "Internal"
        return nc.dram_tensor(name, list(shape), dt, kind=kind).ap()

    xs = din("xs", [VT, D])
    memb = din("memb", [256, D])
    w_in = din("w_in", [D, DIN])
    w_kv = din("w_mem_kv", [D, 512])
    w_ba = din("w_branch_a", [512, D])
    w_bb = din("w_branch_b", [512, D])
    w_bm = din("w_branch_m", [256, D])
    w_o = din("w_out", [D, D])
    nw_d = din("nw", [128, 8])
    mnw_d = din("mnw", [128, 8])
    bmg_d = din("bmg", [128, 24])
    fnw_d = din("fnw", [128, D])
    ident_d = din("ident", [128, 128], BF16)
    identf_d = din("identf", [128, 128])
    bm_d = din("bm", [128, 3 * 8 * 256])
    sel_d = din("sel", [64, VT], BF16)
    ab_d = din("ab", [128, 8 * NM])
    ct_d = din("ct", [128, 32])
    pm_d = din("pm", [128, 128])
    cb_d = din("cb", [128, 4 * 512])
    vb_d = din("vb", [128, 64])
    hv_d = din("hv", [128, 1])
    out_d = nc.dram_tensor("out", [T, D], F32, kind="ExternalOutput").ap()

    KTb = dscr("KTb", [512, VT], BF16)
    Vb = dscr("Vb", [VT, 512], BF16)
    QTb = dscr("QTb", [512, T], BF16)
    QTg = [dscr("QTg%d" % g, [512, T], BF16) for g in range(3)]
    KTg = [dscr("KTg%d" % g, [512, HT], BF16) for g in range(3)]
    Vg = [dscr("Vg%d" % g, [HT, 512], BF16) for g in range(3)]
    Ga = dscr("Ga", [T, 512], BF16)
    Gb = dscr("Gb", [T, 512], BF16)
    Gm = dscr("Gm", [T, 256], BF16)
    MQT = dscr("MQT", [256, T], BF16)
    SG = dscr("SG", [3072, T], BF16)
    OD = [dscr("OD%d" % g, [T, 8, 65], F32) for g in range(3)]
    OB = dscr("OB", [T, 8, 65], F32)
    OM = dscr("OM", [T, 256], F32)

    es = contextlib.ExitStack()

    def sb(stack, name, shape, dt):
        return stack.enter_context(nc.sbuf_tensor("s_" + name, list(shape), dt))

    with es:
        ident = sb(es, "ident", [128, 128], BF16)
        identf = sb(es, "identf", [128, 128], F32)
        nw = sb(es, "nw", [128, 8], F32)
        mnw = sb(es, "mnw", [128, 8], F32)
        hv = sb(es, "hv", [128, 1], F32)
        PS = [es.enter_context(nc.psum_tensor("ps%d" % i, [128, 512], F32)) for i in range(6)]
        PB = [es.enter_context(nc.psum_tensor("pb%d" % i, [128, 1024], BF16)) for i in range(2)]
        for i, (dst, src) in enumerate([(ident, ident_d), (identf, identf_d), (nw, nw_d), (mnw, mnw_d), (hv, hv_d)]):
            tr.dma("sp", dst[:], src, writes=["c%d" % i], slot="ld%d" % (i % 2))
        tr.barrier()

        evac_i = [0]

        def evac(out, in_, rd, wr, func=None, bias=None, eng=None):
            if func is not None:
                e = "act"
            elif eng is not None:
                e = eng
            else:
                e = "act" if evac_i[0] % 2 == 0 else "dve"
                evac_i[0] += 1
            if e == "act":
                if func is None:
                    tr.op("act", lambda: nc.scalar.copy(out=out, in_=in_), reads=rd, writes=wr)
                elif bias is None:
                    tr.op("act", lambda: nc.scalar.activation(out=out, in_=in_, func=func), reads=rd, writes=wr)
                else:
                    tr.op("act", lambda: nc.scalar.activation(out=out, in_=in_, func=func, bias=bias), reads=rd, writes=wr)
            else:
                tr.op("dve", lambda: nc.vector.tensor_copy(out=out, in_=in_), reads=rd, writes=wr)

        cv_i = [0]

        def conv_scale(out, in_, sc, rd, wr):
            e = ("dve", "pool", "act")[cv_i[0] % 3]
            cv_i[0] += 1
            if e == "act":
                if sc is None:
                    tr.op("act", lambda: nc.scalar.copy(out=out, in_=in_), reads=rd, writes=wr)
                else:
                    tr.op("act", lambda: nc.scalar.activation(out=out, in_=in_, func=AF.Copy, scale=sc), reads=rd, writes=wr)
            else:
                en = nc.vector if e == "dve" else nc.gpsimd
                if sc is None:
                    tr.op(e, lambda: en.tensor_copy(out=out, in_=in_), reads=rd, writes=wr)
                else:
                    tr.op(e, lambda: en.tensor_scalar(out=out, in0=in_, scalar1=sc, scalar2=None, op0=ALU.mult),
                          reads=rd, writes=wr)

        def rms_tile(st, xt, xres, nsub, hb, hbres, ssb, ssres):
            tr.op("dve", lambda: nc.vector.memset(ssb[:], 0.0), writes=[ssres])
            junk = st["junk"]
            for s in range(nsub):
                tr.op("act", lambda s=s: nc.scalar.activation(out=junk[:], in_=xt[:, s, :], func=AF.Square,
                                                               accum_out=ssb[:, s:s + 1]),
                      reads=[xres, ssres], writes=["junk", ssres])
            tr.op("act", lambda: nc.scalar.activation(out=ssb[:, nsub:2 * nsub], in_=ssb[:, 0:nsub], func=AF.Sqrt,
                                                       bias=EPS, scale=1.0 / D), reads=[ssres], writes=[ssres])
            tr.op("dve", lambda: nc.vector.reciprocal(out=ssb[:, 2 * nsub:3 * nsub], in_=ssb[:, nsub:2 * nsub]),
                  reads=[ssres], writes=[ssres])
            for s in range(nsub):
                sc = ssb[:, 2 * nsub + s:2 * nsub + s + 1]
                if s % 2 == 0:
                    tr.op("dve", lambda s=s, sc=sc: nc.vector.tensor_scalar(out=hb[:, s, :], in0=xt[:, s, :], scalar1=sc,
                                                                           scalar2=None, op0=ALU.mult),
                          reads=[xres, ssres], writes=[hbres])
                else:
                    tr.op("act", lambda s=s, sc=sc: nc.scalar.activation(out=hb[:, s, :], in_=xt[:, s, :], func=AF.Copy, scale=sc),
                          reads=[xres, ssres], writes=[hbres])

        with contextlib.ExitStack() as p1:
            hT = sb(p1, "hT", [128, 8, HT], BF16)
            wst = sb(p1, "wst", [128, 8, 512], F32)
            wbf = [sb(p1, "wbf%d" % i, [128, 8, 512], BF16) for i in range(2)]
            junk = sb(p1, "junk", [128, D], BF16)
            st = {"junk": junk}
            bmg = sb(p1, "bmg", [128, 24], F32)
            tr.dma("sp", bmg[:], bmg_d, writes=["bmg"], slot="ld0")

            def load_w(col0, ncols, dst, dres, src=w_in, scale=nw):
                tr.dma("sp", wst[:, :, 0:ncols], src[:, col0:col0 + ncols].rearrange("(fc p) c -> p fc c", p=128),
                       writes=["wst"], slot="wst")
                for fc in range(8):
                    conv_scale(dst[:, fc, 0:ncols], wst[:, fc, 0:ncols], scale[:, fc:fc + 1], ["wst"], [dres])

            with contextlib.ExitStack() as p1a:
                xts = Rot([sb(p1a, "xt%d" % i, [128, 2, D], F32) for i in range(4)], "xt")
                hbs = Rot([sb(p1a, "hb%d" % i, [128, 2, D], BF16) for i in range(2)], "hb")
                sss = Rot([sb(p1a, "ss%d" % i, [128, 6], F32) for i in range(2)], "ss")
                hts = Rot([sb(p1a, "htt%d" % i, [128, 8, 256], BF16) for i in range(2)], "htt")
                ksts = Rot([sb(p1a, "kst%d" % i, [128, 4, 256], BF16) for i in range(2)], "kst")
                vsts = Rot([sb(p1a, "vst%d" % i, [128, 2, 512], BF16) for i in range(2)], "vst")
                load_w(5632, 512, wbf[0], "wbf0")
                load_w(6144, 512, wbf[1], "wbf1")
                NT1 = VT // 256
                pbi = 0
                psi = 0
                nst = {}

                xld = {}

                def stage_l(i):
                    xt, xres = xts.next()
                    tr.dma("sp", xt[:], xs[256 * i:256 * i + 256, :].rearrange("(s p) f -> p s f", p=128),
                           writes=[xres], slot=xres)
                    xld[i] = (xt, xres)

                def stage_n(i):
                    if i + 2 < NT1:
                        stage_l(i + 2)
                    xt, xres = xld.pop(i)
                    hb, hbres = hbs.next()
                    ssb, ssres = sss.next()
                    rms_tile(st, xt, xres, 2, hb, hbres, ssb, ssres)
                    nst[i] = (hb, hbres)

                mst = {}
                cnt1 = {"pbi": 0, "psi": 0}

                def stage_mt(i):
                    hb, hbres = nst.pop(i)
                    resident = (256 * i >= VT - HT)
                    if resident:
                        u0 = 256 * i - (VT - HT)
                        hdst = lambda fc, u0=u0: hT[:, fc, u0:u0 + 256]
                        hres = "hTres%d" % i
                    else:
                        htt, hres = hts.next()
                        hdst = lambda fc, htt=htt: htt[:, fc, :]
                    mst[i] = (hdst, hres)
                    for fc in range(8):
                        pb = PB[cnt1["pbi"] % 2]
                        pbres = "pb%d" % (cnt1["pbi"] % 2)
                        cnt1["pbi"] += 1
                        for s in range(2):
                            tr.op("pe", lambda s=s, fc=fc, pb=pb: nc.tensor.transpose(
                                out=pb[:, s * 128:(s + 1) * 128], in_=hb[:, s, fc * 128:(fc + 1) * 128], identity=ident[:]),
                                reads=[hbres], writes=[pbres])
                        evac(hdst(fc), pb[:, 0:256], [pbres], [hres])

                def stage_mm(i):
                    hdst, hres = mst.pop(i)
                    kst, kres = ksts.next()
                    for pr in range(4):
                        ps = PS[cnt1["psi"] % 6]
                        psres = "ps%d" % (cnt1["psi"] % 6)
                        cnt1["psi"] += 1
                        for fc in range(8):
                            tr.op("pe", lambda fc=fc, pr=pr, ps=ps: nc.tensor.matmul(
                                ps[:, 0:256], lhsT=wbf[0][:, fc, pr * 128:(pr + 1) * 128], rhs=hdst(fc),
                                start=(fc == 0), stop=(fc == 7)), reads=["wbf0", hres], writes=[psres])
                        evac(kst[:, pr, :], ps[:, 0:256], [psres], [kres])
                    tr.dma("pool", KTb[:, 256 * i:256 * i + 256].rearrange("(pr p) t -> p pr t", p=128), kst[:],
                           reads=[kres], writes=[], slot=kres)
                    vst, vres = vsts.next()
                    for s in range(2):
                        ps = PS[cnt1["psi"] % 6]
                        psres = "ps%d" % (cnt1["psi"] % 6)
                        cnt1["psi"] += 1
                        for fc in range(8):
                            tr.op("pe", lambda fc=fc, s=s, ps=ps: nc.tensor.matmul(
                                ps[:, :], lhsT=hdst(fc)[:, s * 128:(s + 1) * 128], rhs=wbf[1][:, fc, :],
                                start=(fc == 0), stop=(fc == 7)), reads=["wbf1", hres], writes=[psres])
                        evac(vst[:, s, :], ps[:, :], [psres], [vres])
                    tr.dma("pool", Vb[256 * i:256 * i + 256, :].rearrange("(s p) c -> p s c", p=128), vst[:],
                           reads=[vres], writes=[], slot=vres)

                stage_l(0)
                stage_l(1)
                stage_n(0)
                stage_n(1)
                stage_mt(0)
                for i in range(NT1):
                    if i + 1 < NT1:
                        stage_mt(i + 1)
                    if i + 2 < NT1:
                        stage_n(i + 2)
                    stage_mm(i)
                tr.barrier()

            with contextlib.ExitStack() as p1b:
                fsts = Rot([sb(p1b, "fst%d" % i, [128, 512], BF16) for i in range(4)], "fst")
                psi = [0]

                blocks = []

                def fm_block(col0, ncols, tiles, func=None, bias_col0=None, deint=0):
                    def units(wb, wres):
                        us = []
                        for cc in range(ncols // 128):
                            for (tsl, N, dstf) in tiles:
                                def u(cc=cc, tsl=tsl, N=N, dstf=dstf):
                                    ps = PS[psi[0] % 6]
                                    psres = "ps%d" % (psi[0] % 6)
                                    psi[0] += 1
                                    for fc in range(8):
                                        tr.op("pe", lambda fc=fc: nc.tensor.matmul(
                                            ps[:, 0:N], lhsT=wb[:, fc, cc * 128:(cc + 1) * 128], rhs=hT[:, fc, tsl],
                                            start=(fc == 0), stop=(fc == 7)), reads=[wres], writes=[psres])
                                    fst, fres = fsts.next()
                                    b = None if bias_col0 is None else bmg[:, bias_col0 + cc:bias_col0 + cc + 1]
                                    if deint:
                                        fv = fst[:, 0:N].rearrange("p (r i) -> p r i", r=deint)
                                        evac(fv, ps[:, 0:N].rearrange("p (i r) -> p r i", r=deint), [psres], [fres])
                                        tr.dma("pool" if psi[0] % 2 else "sp", dstf(cc), fv, reads=[fres], writes=[], slot=fres)
                                        return
                                    evac(fst[:, 0:N], ps[:, 0:N], [psres], [fres], func=func, bias=b)
                                    tr.dma("pool" if psi[0] % 2 else "sp", dstf(cc), fst[:, 0:N], reads=[fres], writes=[], slot=fres)
                                us.append(u)
                        return us
                    blocks.append((col0, ncols, units))

                def tm_block(col0, ncols, tiles, func=None):
                    def units(wb, wres):
                        us = []
                        for (tsl, dst) in tiles:
                            def u(tsl=tsl, dst=dst):
                                ps = PS[psi[0] % 6]
                                psres = "ps%d" % (psi[0] % 6)
                                psi[0] += 1
                                for fc in range(8):
                                    tr.op("pe", lambda fc=fc: nc.tensor.matmul(
                                        ps[:, 0:ncols], lhsT=hT[:, fc, tsl], rhs=wb[:, fc, 0:ncols],
                                        start=(fc == 0), stop=(fc == 7)), reads=[wres], writes=[psres])
                                fst, fres = fsts.next()
                                evac(fst[:, 0:ncols], ps[:, 0:ncols], [psres], [fres], func=func)
                                tr.dma("pool" if psi[0] % 2 else "sp", dst, fst[:, 0:ncols], reads=[fres], writes=[], slot=fres)
                            us.append(u)
                        return us
                    blocks.append((col0, ncols, units))

                own_fm = lambda dst: [(slice(HALO + 512 * i, HALO + 512 * i + 512), 512,
                                       (lambda cc, i=i: dst[cc * 128:(cc + 1) * 128, 512 * i:512 * i + 512]))
                                      for i in range(8)]
                own_tm = lambda dst, nco: [(slice(HALO + 128 * i, HALO + 128 * i + 128), dst[128 * i:128 * i + 128, 0:nco])
                                           for i in range(32)]
                for g, d in enumerate(DILS):
                    c0 = 1536 * g
                    nq = T // d
                    nk = HT // d
                    if d == 1:
                        tiles = [(slice(HALO + 512 * i, HALO + 512 * i + 512), 512,
                                  (lambda cc, i=i, g=g: QTg[g][cc * 128:(cc + 1) * 128, 512 * i:512 * i + 512])) for i in range(8)]
                        fm_block(c0, 512, tiles)
                        i_first = HALO - 128
                        tiles = []
                        i0 = i_first
                        while i0 < nk:
                            N = min(512, nk - i0)
                            tiles.append((slice(i0, i0 + N), N,
                                          (lambda cc, i0=i0, N=N, g=g: KTg[g][cc * 128:(cc + 1) * 128, i0:i0 + N])))
                            i0 += N
                        fm_block(c0 + 512, 512, tiles)
                    else:
                        w = 512 // d
                        tiles = [(slice(HALO + 512 * i, HALO + 512 * i + 512), 512,
                                  (lambda cc, i=i, g=g, d=d, w=w: QTg[g][cc * 128:(cc + 1) * 128, :].rearrange(
                                      "p (r i) -> p r i", r=d)[:, :, w * i:w * i + w])) for i in range(8)]
                        fm_block(c0, 512, tiles, deint=d)
                        u_first = HALO - 128 * d
                        tiles = [(slice(u0, u0 + 512), 512,
                                  (lambda cc, u0=u0, g=g, d=d, w=w: KTg[g][cc * 128:(cc + 1) * 128, :].rearrange(
                                      "p (r i) -> p r i", r=d)[:, :, u0 // d:u0 // d + w])) for u0 in range(u_first, HT, 512)]
                        fm_block(c0 + 512, 512, tiles, deint=d)
                    tiles = []
                    for r in range(d):
                        for kb in range(HALO // d // 128 - 1, nk // 128):
                            u0 = r + d * 128 * kb
                            p0 = r * nk + 128 * kb
                            tiles.append((slice(u0, u0 + d * 127 + 1, d), Vg[g][p0:p0 + 128, :]))
                    tm_block(c0 + 1024, 512, tiles)
                tm_block(4608, 512, own_tm(Ga, 512), func=AF.Silu)
                fm_block(5120, 512, own_fm(QTb))
                tm_block(6656, 512, own_tm(Gb, 512), func=AF.Silu)
                fm_block(7168, 256, own_fm(MQT))
                tm_block(7424, 256, own_tm(Gm, 256), func=AF.Silu)
                for j in range(6):
                    fm_block(7680 + 512 * j, 512,
                             [(sl, N, (lambda cc, f=f, j=j: f(cc + 4 * j))) for (sl, N, f) in own_fm(SG)],
                             func=AF.Sigmoid, bias_col0=4 * j)

                def w_dma(n):
                    col0, ncols, _ = blocks[n]
                    tr.dma("sp", wst[:, :, 0:ncols], w_in[:, col0:col0 + ncols].rearrange("(fc p) c -> p fc c", p=128),
                           writes=["wst"], slot="wst")

                def w_conv(n):
                    col0, ncols, _ = blocks[n]
                    k = n % 2
                    for fc in range(8):
                        conv_scale(wbf[k][:, fc, 0:ncols], wst[:, fc, 0:ncols], nw[:, fc:fc + 1], ["wst"], ["wbf%d" % k])

                w_dma(0)
                w_conv(0)
                for n in range(len(blocks)):
                    if n + 1 < len(blocks):
                        w_dma(n + 1)
                    us = blocks[n][2](wbf[n % 2], "wbf%d" % (n % 2))
                    for ui, u in enumerate(us):
                        if ui == (2 * len(us)) // 3 and n + 1 < len(blocks):
                            w_conv(n + 1)
                        u()
                tr.barrier()

        if "stop1" in dbg:
            tr.finish()
            return nc

        with contextlib.ExitStack() as p2:
            bm = sb(p2, "bm", [128, 3 * 8 * 256], F32)
            tr.dma("sp", bm[:], bm_d, writes=["bm"], slot="ld0")
            qts = Rot([sb(p2, "qt%d" % i, [128, T], BF16) for i in range(2)], "qt")
            kts = Rot([sb(p2, "kt%d" % i, [128, HT], BF16) for i in range(2)], "kt")
            vas = Rot([sb(p2, "va%d" % i, [128, 48, 2, 65], BF16) for i in range(2)], "va")
            efs = Rot([sb(p2, "ef%d" % i, [128, 256], F32) for i in range(3)], "ef")
            ems = Rot([sb(p2, "em%d" % i, [128, 256], BF16) for i in range(4)], "em")
            osbs = Rot([sb(p2, "osb%d" % i, [128, 32, 2, 65], F32) for i in range(2)], "osb")
            for va in vas.tiles:
                tr.op("pool", lambda va=va: nc.gpsimd.memset(va[:, :, :, 64:65], 1.0), writes=["va0", "va1"])
            tr.barrier()
            for g, d in enumerate(DILS):
                nk = HT // d
                nq = T // d
                nkb = nk // 128
                qb0 = nkb - nq // 128
                nqo = nq // 128
                for hp in range(4):
                    qt, qres = qts.next()
                    kt, kres = kts.next()
                    va, vres = vas.next()
                    tr.dma("sp", qt[:], QTg[g][hp * 128:(hp + 1) * 128, :], writes=[qres], slot=qres)
                    tr.dma("sp", kt[:], KTg[g][hp * 128:(hp + 1) * 128, :], writes=[kres], slot=kres)
                    for hh in range(2):
                        for part in range(2):
                            tr.dma("sp", va[:, 24 * part:24 * part + 24, hh, 0:64],
                                   Vg[g][3072 * part:3072 * part + 3072, hp * 128 + 64 * hh:hp * 128 + 64 * hh + 64].rearrange(
                                       "(c p) e -> p c e", p=128),
                                   writes=[vres], slot="%sp%d" % (vres, part % 2))
                    steps = []
                    for r in range(d):
                        osb, ores = osbs.next()
                        for hh in range(2):
                            for kb in range(qb0 - 1, nkb):
                                has_cur = kb >= qb0
                                has_prev = kb + 1 < nkb
                                if not (has_cur or has_prev):
                                    continue
                                steps.append(dict(r=r, hh=hh, kb=kb, has_cur=has_cur, has_prev=has_prev, osb=osb, ores=ores,
                                                  last=(hh == 1 and kb == nkb - 1)))
                    ns = len(steps)

                    def stage_a(i):
                        sp_ = steps[i]
                        r, hh, kb, has_cur, has_prev = sp_["r"], sp_["hh"], sp_["kb"], sp_["has_cur"], sp_["has_prev"]
                        h = 2 * hp + hh
                        rows = slice(64 * hh, 64 * hh + 64)
                        bmo = (g * 8 + h) * 256
                        qlo = (kb if has_cur else kb + 1) - qb0
                        qhi = (kb + 1 if has_prev else kb) - qb0
                        ncol = 128 * (qhi - qlo + 1)
                        qc0 = r * nq + 128 * qlo
                        kc0 = r * nk + 128 * kb
                        ps_s = PS[i % 3]
                        ps_sres = "ps%d" % (i % 3)
                        tr.op("pe", lambda: nc.tensor.matmul(
                            ps_s[:, 0:ncol], lhsT=kt[rows, kc0:kc0 + 128], rhs=qt[rows, qc0:qc0 + ncol],
                            start=True, stop=True), reads=[kres, qres], writes=[ps_sres])
                        ef, efres = efs.next()
                        tr.op("act", lambda: nc.scalar.activation(
                            out=ef[:, 0:ncol], in_=ps_s[:, 0:ncol], func=AF.Exp, scale=SCALE),
                            reads=[ps_sres], writes=[efres])
                        em, emres = ems.next()
                        b0 = bmo if has_cur else bmo + 128
                        if kb < qb0:
                            tr.op("dve", lambda: nc.vector.scalar_tensor_tensor(
                                out=em[:, 0:ncol], in0=ef[:, 0:ncol], scalar=hv[:, 0:1], in1=bm[:, b0:b0 + ncol],
                                op0=ALU.mult, op1=ALU.mult), reads=[efres, "bm"], writes=[emres])
                        else:
                            tr.op("dve", lambda: nc.vector.tensor_tensor(
                                out=em[:, 0:ncol], in0=ef[:, 0:ncol], in1=bm[:, b0:b0 + ncol], op=ALU.mult),
                                reads=[efres, "bm"], writes=[emres])
                        sp_["em"] = (em, emres)

                    def stage_b(i):
                        sp_ = steps[i]
                        r, hh, kb, has_cur, has_prev = sp_["r"], sp_["hh"], sp_["kb"], sp_["has_cur"], sp_["has_prev"]
                        osb, ores = sp_["osb"], sp_["ores"]
                        em, emres = sp_["em"]
                        vch = r * nkb + kb
                        col = 0
                        if has_cur:
                            po = PS[3 + (kb % 2)]
                            pores = "ps%d" % (3 + (kb % 2))
                            tr.op("pe", lambda: nc.tensor.matmul(
                                po[:, 0:65], lhsT=em[:, 0:128], rhs=va[:, vch, hh, :], start=False, stop=True),
                                reads=[emres, vres], writes=[pores])
                            qo = kb - qb0
                            evac(osb[:, qo, hh, :], po[:, 0:65], [pores], [ores], eng="dve" if i % 3 else "act")
                            col = 128
                        if has_prev:
                            po = PS[3 + ((kb + 1) % 2)]
                            pores = "ps%d" % (3 + ((kb + 1) % 2))
                            tr.op("pe", lambda: nc.tensor.matmul(
                                po[:, 0:65], lhsT=em[:, col:col + 128], rhs=va[:, vch, hh, :], start=True, stop=False),
                                reads=[emres, vres], writes=[pores])
                        if sp_["last"]:
                            tr.dma("pool", OD[g][r:T:d, 2 * hp:2 * hp + 2, :].rearrange("(qo a) h e -> a qo h e", a=128),
                                   osb[:, 0:nqo, :, :], reads=[ores], writes=[], slot=ores)

                    stage_a(0)
                    if ns > 1:
                        stage_a(1)
                    for i in range(ns):
                        if i + 2 < ns:
                            stage_a(i + 2)
                        stage_b(i)
            tr.barrier()

        if "stop2" in dbg:
            tr.finish()
            return nc

        with contextlib.ExitStack() as p3:
            kas = Rot([sb(p3, "ka%d" % i, [128, VT], BF16) for i in range(2)], "ka")
            qas = Rot([sb(p3, "qa%d" % i, [128, T], BF16) for i in range(2)], "qa")
            vbs_ = Rot([sb(p3, "vv%d" % i, [128, 128, 65], BF16) for i in range(2)], "vv")
            ab = sb(p3, "ab", [128, 8 * NM], F32)
            ct = sb(p3, "ct", [128, 32], F32)
            pm = sb(p3, "pm", [128, 128], F32)
            cb = sb(p3, "cb", [128, 4 * 512], F32)
            vbt = sb(p3, "vbt", [128, 64], F32)
            ksum = sb(p3, "ksum", [64, 64], F32)
            kmb = sb(p3, "kmb", [64, 64], BF16)
            g1s = Rot([sb(p3, "g1%d" % i, [128, 64], F32) for i in range(2)], "g1")
            t8s = Rot([sb(p3, "t8%d" % i, [128, 16], F32) for i in range(2)], "t8")
            sms = Rot([sb(p3, "sm%d" % i, [128, 64], F32) for i in range(2)], "sm")
            mbs = Rot([sb(p3, "mb%d" % i, [128, 128], BF16) for i in range(2)], "mb")
            ets = Rot([sb(p3, "et%d" % i, [128, 512], BF16) for i in range(3)], "et")
            tmps = Rot([sb(p3, "tmpf%d" % i, [128, 512], F32) for i in range(2)], "tmpf")
            otf = sb(p3, "otf", [65, 512], F32)
            obs = Rot([sb(p3, "obst%d" % i, [128, 4, 65], F32) for i in range(2)], "obst")
            for i, (dst, src) in enumerate([(ab, ab_d), (ct, ct_d), (pm, pm_d), (cb, cb_d), (vbt, vb_d)]):
                tr.dma("sp", dst[:], src, writes=["k%d" % i], slot="ld%d" % (i % 2))
            for i, ka in enumerate(kas.tiles):
                tr.dma("sp", ka[64:128, :], sel_d, writes=["ka%d" % i], slot="ld%d" % (i % 2))
            for i, vv in enumerate(vbs_.tiles):
                tr.op("pool", lambda vv=vv: nc.gpsimd.memset(vv[:, :, 64:65], 1.0), writes=["vv%d" % i])
            for i, mb in enumerate(mbs.tiles):
                tr.op("pool", lambda mb=mb: nc.gpsimd.memset(mb[:, 0:64], 0.0), writes=["mb%d" % i])
            tr.barrier()
            otfs = Rot([otf, sb(p3, "otf1", [65, 512], F32)], "otf")
            bufs = {}

            def loads(h):
                ka, kares = kas.next()
                qa, qares = qas.next()
                vv, vvres = vbs_.next()
                bufs[h] = (ka, kares, qa, qares, vv, vvres)
                for part in range(4):
                    tr.dma("sp", ka[0:64, 4096 * part:4096 * part + 4096],
                           KTb[64 * h:64 * h + 64, 4096 * part:4096 * part + 4096],
                           writes=[kares], slot="%sp%d" % (kares, part % 2))
                tr.dma("sp", qa[0:64, :], QTb[64 * h:64 * h + 64, :], writes=[qares + "q"], slot=qares)
                for part in range(8):
                    tr.dma("sp", vv[:, 16 * part:16 * part + 16, 0:64],
                           Vb[2048 * part:2048 * part + 2048, 64 * h:64 * h + 64].rearrange("(c p) e -> p c e", p=128),
                           writes=[vvres], slot="%sp%d" % (vvres, part % 2))

            def gate_tasks(h):
                ka, kares, qa, qares, vv, vvres = bufs[h]
                tasks = []

                def t_kmean():
                    tr.op("dve", lambda: nc.vector.tensor_reduce(
                        out=ksum[:, :], in_=ka[0:64, :].rearrange("p (n k) -> p n k", k=256), axis=AX.X, op=ALU.add),
                        reads=[kares], writes=["ksum"])
                    tr.op("dve", lambda: nc.vector.tensor_copy(out=kmb[:, :], in_=ksum[:, :]), reads=["ksum"], writes=["kmb"])
                tasks.append(t_kmean)
                st = {}
                for qi in range(32):
                    def ta(qi=qi):
                        b_own = (VT - T) // 256 + qi // 2
                        pg = PS[5]
                        tr.op("pe", lambda: nc.tensor.matmul(
                            pg[:, 0:64], lhsT=qa[0:64, 128 * qi:128 * qi + 128], rhs=kmb[:, :], start=True, stop=True),
                            reads=[qares + "q", "kmb"], writes=["ps5"])
                        g1, g1res = g1s.next()
                        tr.op("dve", lambda: nc.vector.tensor_tensor(
                            out=g1[:], in0=pg[:, 0:64], in1=pm[:, 64 - b_own:128 - b_own], op=ALU.add),
                            reads=["ps5"], writes=[g1res])
                        tr.op("dve", lambda: nc.vector.tensor_tensor(out=g1[:], in0=g1[:], in1=vbt[:], op=ALU.add),
                              reads=[g1res], writes=[g1res])
                        t8, t8res = t8s.next()
                        tr.op("dve", lambda: nc.vector.max(out=t8[:, 0:8], in_=g1[:]), reads=[g1res], writes=[t8res])
                        tr.op("dve", lambda: nc.vector.tensor_scalar_max(out=t8[:, 8:9], in0=t8[:, 2:3], scalar1=-1e29),
                              reads=[t8res], writes=[t8res])
                        sm, smres = sms.next()
                        tr.op("dve", lambda: nc.vector.tensor_scalar(
                            out=sm[:], in0=g1[:], scalar1=t8[:, 8:9], scalar2=None, op0=ALU.is_ge),
                            reads=[g1res, t8res], writes=[smres])
                        tr.op("dve", lambda: nc.vector.memset(sm[:, b_own:b_own + 1], 1.0), reads=[smres], writes=[smres])
                        mb, mbres = mbs.next()
                        cti = 4 * h + qi % 4
                        tr.op("dve", lambda: nc.vector.tensor_scalar(
                            out=mb[:, 64:128], in0=sm[:], scalar1=ct[:, cti:cti + 1], scalar2=MBNEG, op0=ALU.mult, op1=ALU.add),
                            reads=[smres], writes=[mbres])
                        st[qi] = (mb, mbres)

                    def tb(qi=qi):
                        mb, mbres = st[qi]
                        pb = PB[qi % 2]
                        pbres = "pb%d" % (qi % 2)
                        tr.op("pe", lambda: nc.tensor.transpose(out=pb[:, 0:128], in_=mb[:, :], identity=ident[:]),
                              reads=[mbres], writes=[pbres])
                        tr.op("dve", lambda: nc.vector.tensor_copy(
                            out=qa[64:128, 128 * qi:128 * qi + 128], in_=pb[64:128, 0:128]),
                            reads=[pbres], writes=[qares + "m%d" % (qi // 4)])
                    tasks.append(ta)
                    tasks.append(tb)
                order = [tasks[0], tasks[1]]
                for qi in range(32):
                    if qi + 1 < 32:
                        order.append(tasks[1 + 2 * (qi + 1)])
                    order.append(tasks[2 + 2 * qi])
                return order

            def main_loop(h, pending):
                ka, kares, qa, qares, vv, vvres = bufs[h]
                items = []
                firsts = {}
                for qt_ in range(8):
                    nch = (VT - T) // 128 + 4 * (qt_ + 1)
                    c_lo = 0
                    if SKIP_FAR:
                        slope = 2.0 ** (-(h + 1))
                        dmin = SKIP_NATS / slope
                        t0q = (VT - T) + 512 * qt_
                        c_lo = max(0, int(np.floor((t0q - 127 - dmin) / 128.0)) + 1)
                    firsts[qt_] = c_lo
                    for c in range(c_lo, nch):
                        items.append((qt_, c, nch))
                n = len(items)
                ets_ = {}
                deferred = {}
                every = max(1, (n - 40) // max(1, len(pending)))

                def stage_a(i):
                    qt_, c, nch = items[i]
                    mi = c - 4 * qt_ + 28
                    ps_s = PS[i % 3]
                    ps_sres = "ps%d" % (i % 3)
                    tr.op("pe", lambda: nc.tensor.matmul(
                        ps_s[:, :], lhsT=ka[:, 128 * c:128 * c + 128], rhs=qa[:, 512 * qt_:512 * qt_ + 512],
                        start=True, stop=True), reads=[kares, qares + "q", qares + "m%d" % qt_], writes=[ps_sres])
                    et, etres = ets.next()
                    ets_[i] = (et, etres)
                    bias = ab[:, h * NM + mi:h * NM + mi + 1]
                    dc = c - (nch - 4)
                    if dc < 0:
                        tr.op("act", lambda: nc.scalar.activation(
                            out=et[:], in_=ps_s[:, :], func=AF.Exp, bias=bias, scale=SCALE),
                            reads=[ps_sres], writes=[etres])
                    else:
                        tmpf, tres = tmps.next()
                        tr.op("dve", lambda: nc.vector.tensor_tensor(
                            out=tmpf[:], in0=ps_s[:, :], in1=cb[:, 512 * dc:512 * dc + 512], op=ALU.add),
                            reads=[ps_sres], writes=[tres])
                        tr.op("act", lambda: nc.scalar.activation(
                            out=et[:], in_=tmpf[:], func=AF.Exp, bias=bias, scale=SCALE),
                            reads=[tres], writes=[etres])

                def stage_b(i):
                    qt_, c, nch = items[i]
                    et, etres = ets_.pop(i)
                    po = PS[3 + qt_ % 2]
                    pores = "ps%d" % (3 + qt_ % 2)
                    tr.op("pe", lambda: nc.tensor.matmul(
                        po[0:65, :], lhsT=vv[:, c, :], rhs=et[:], start=(c == firsts[qt_]), stop=(c == nch - 1)),
                        reads=[etres, vvres], writes=[pores])
                    if c == nch - 1:
                        of, ofres = otfs.next()
                        tr.op("dve", lambda: nc.vector.tensor_copy(out=of[:, :], in_=po[0:65, :]), reads=[pores], writes=[ofres])

                        def fin(qt_=qt_, of=of, ofres=ofres):
                            px = PS[5]
                            for s in range(4):
                                tr.op("pe", lambda s=s: nc.tensor.transpose(
                                    out=px[:, 65 * s:65 * s + 65], in_=of[0:65, 128 * s:128 * s + 128],
                                    identity=identf[0:65, 0:65]), reads=[ofres], writes=["ps5"])
                            obst, obres = obs.next()
                            tr.op("dve", lambda: nc.vector.tensor_copy(
                                out=obst[:].rearrange("p s e -> p (s e)"), in_=px[:, 0:260]), reads=["ps5"], writes=[obres])
                            tr.dma("pool", OB[512 * qt_:512 * qt_ + 512, h, :].rearrange("(s p) e -> p s e", p=128), obst[:],
                                   reads=[obres], writes=[], slot=obres)
                        deferred[min(n - 1, i + 4)] = fin

                stage_a(0)
                stage_a(1)
                for i in range(n):
                    if i + 2 < n:
                        stage_a(i + 2)
                    stage_b(i)
                    if i in deferred:
                        deferred.pop(i)()
                    if pending and i >= 8 and (i - 8) % every == 0:
                        pending.pop(0)()
                for k in sorted(deferred):
                    deferred[k]()
                while pending:
                    pending.pop(0)()

            horder = [7, 6, 5, 4, 3, 2, 1, 0]
            loads(horder[0])
            for t in gate_tasks(horder[0]):
                t()
            for hi, h in enumerate(horder):
                pending = []
                if hi + 1 < 8:
                    loads(horder[hi + 1])
                    pending = gate_tasks(horder[hi + 1])
                main_loop(h, pending)
            tr.barrier()

        if "stop3" in dbg:
            tr.finish()
            return nc

        with contextlib.ExitStack() as p4:
            wst = sb(p4, "wst4", [128, 8, 512], F32)
            wkv = sb(p4, "wkv", [128, 8, 512], BF16)
            mx = sb(p4, "mx", [128, 2, D], F32)
            mhb = sb(p4, "mhb", [128, 2, D], BF16)
            junk = sb(p4, "junk4", [128, D], BF16)
            mss = sb(p4, "mss", [128, 6], F32)
            memT = sb(p4, "memT", [128, 8, 256], BF16)
            mkT = sb(p4, "mkT", [128, 2, 256], BF16)
            mva = sb(p4, "mva", [128, 2, 4, 65], BF16)
            mq = sb(p4, "mq", [128, 2, T], BF16)
            ems = Rot([sb(p4, "em4%d" % i, [128, 2, 512], BF16) for i in range(2)], "em4")
            rds = Rot([sb(p4, "rd4%d" % i, [128, 4], F32) for i in range(2)], "rd4")
            oms = Rot([sb(p4, "om4%d" % i, [128, 4, 64], F32) for i in range(2)], "om4")
            tr.dma("sp", wst[:], w_kv.rearrange("(fc p) c -> p fc c", p=128), writes=["wst4"], slot="ld0")
            tr.dma("sp", mx[:], memb.rearrange("(s p) f -> p s f", p=128), writes=["mx"], slot="ld1")
            tr.dma("sp", mq[:], MQT.rearrange("(pr p) t -> p pr t", p=128), writes=["mq"], slot="ld0")
            tr.op("pool", lambda: nc.gpsimd.memset(mva[:, :, :, 64:65], 1.0), writes=["mva1"])
            for fc in range(8):
                conv_scale(wkv[:, fc, :], wst[:, fc, :], mnw[:, fc:fc + 1], ["wst4"], ["wkv"])
            rms_tile({"junk": junk}, mx, "mx", 2, mhb, "mhb", mss, "mss")
            for fc in range(8):
                pb = PB[fc % 2]
                pbres = "pb%d" % (fc % 2)
                for s in range(2):
                    tr.op("pe", lambda s=s, fc=fc, pb=pb: nc.tensor.transpose(
                        out=pb[:, s * 128:(s + 1) * 128], in_=mhb[:, s, fc * 128:(fc + 1) * 128], identity=ident[:]),
                        reads=["mhb"], writes=[pbres])
                evac(memT[:, fc, :], pb[:, 0:256], [pbres], ["memT"])
            for pr in range(2):
                ps = PS[pr]
                for fc in range(8):
                    tr.op("pe", lambda fc=fc, pr=pr, ps=ps: nc.tensor.matmul(
                        ps[:, 0:256], lhsT=wkv[:, fc, pr * 128:(pr + 1) * 128], rhs=memT[:, fc, :],
                        start=(fc == 0), stop=(fc == 7)), reads=["wkv", "memT"], writes=["ps%d" % pr])
                evac(mkT[:, pr, :], ps[:, 0:256], ["ps%d" % pr], ["mkT"])
            for s in range(2):
                ps = PS[2 + s]
                for fc in range(8):
                    tr.op("pe", lambda fc=fc, s=s, ps=ps: nc.tensor.matmul(
                        ps[:, 0:256], lhsT=memT[:, fc, s * 128:(s + 1) * 128], rhs=wkv[:, fc, 256:512],
                        start=(fc == 0), stop=(fc == 7)), reads=["wkv", "memT"], writes=["ps%d" % (2 + s)])
                evac(mva[:, s, :, 0:64], ps[:, 0:256].rearrange("p (h e) -> p h e", e=64), ["ps%d" % (2 + s)], ["mva"])
            it = 0
            for h in range(4):
                pr = h // 2
                rows = slice(64 * (h % 2), 64 * (h % 2) + 64)
                for qt_ in range(8):
                    em, emres = ems.next()
                    for mc in range(2):
                        ps_s = PS[mc]
                        tr.op("pe", lambda ps_s=ps_s, mc=mc, qt_=qt_, pr=pr, rows=rows: nc.tensor.matmul(
                            ps_s[:, :], lhsT=mkT[rows, pr, 128 * mc:128 * mc + 128], rhs=mq[rows, pr, 512 * qt_:512 * qt_ + 512],
                            start=True, stop=True), reads=["mkT", "mq"], writes=["ps%d" % mc])
                        tr.op("act", lambda em=em, ps_s=ps_s, mc=mc: nc.scalar.activation(
                            out=em[:, mc, :], in_=ps_s[:, :], func=AF.Exp, scale=SCALE), reads=["ps%d" % mc], writes=[emres])
                    po = PS[2 + it % 2]
                    pores = "ps%d" % (2 + it % 2)
                    it += 1
                    for s in range(4):
                        for mc in range(2):
                            tr.op("pe", lambda po=po, s=s, mc=mc, em=em, h=h: nc.tensor.matmul(
                                po[:, 65 * s:65 * s + 65], lhsT=em[:, mc, 128 * s:128 * s + 128], rhs=mva[:, mc, h, :],
                                start=(mc == 0), stop=(mc == 1)), reads=[emres, "mva", "mva1"], writes=[pores])
                    rd, rdres = rds.next()
                    pov = po[:, 0:260].rearrange("p (s e) -> p s e", e=65)
                    tr.op("dve", lambda rd=rd, pov=pov: nc.vector.reciprocal(out=rd[:], in_=pov[:, :, 64]),
                          reads=[pores], writes=[rdres])
                    om, omres = oms.next()
                    tr.op("dve", lambda om=om, rd=rd, pov=pov: nc.vector.tensor_tensor(
                        out=om[:], in0=pov[:, :, 0:64], in1=rd[:].unsqueeze(2).to_broadcast([128, 4, 64]), op=ALU.mult),
                        reads=[pores, rdres], writes=[omres])
                    tr.dma("pool", OM[512 * qt_:512 * qt_ + 512, 64 * h:64 * h + 64].rearrange("(s p) e -> p s e", p=128),
                           om[:], reads=[omres], writes=[], slot=omres)
            tr.barrier()

        if "stop4" in dbg:
            tr.finish()
            return nc

        with contextlib.ExitStack() as p5:
            wst = sb(p5, "wst5", [128, 2, D], F32)
            wbr = sb(p5, "wbr", [128, 10, D], BF16)
            wout = sb(p5, "wout", [128, 8, D], BF16)
            fnw = sb(p5, "fnw", [128, D], F32)
            tr.dma("sp", fnw[:], fnw_d, writes=["fnw"], slot="ld1")
            for (src, nfc, f0, dstw) in ((w_ba, 4, 0, wbr), (w_bb, 4, 4, wbr), (w_bm, 2, 8, wbr), (w_o, 8, 0, wout)):
                for f2 in range(0, nfc, 2):
                    tr.dma("sp", wst[:], src[128 * f2:128 * f2 + 256, :].rearrange("(fc p) c -> p fc c", p=128),
                           writes=["wst5"], slot="ld0")
                    for k in range(2):
                        conv_scale(dstw[:, f0 + f2 + k, :], wst[:, k, :], None, ["wst5"], ["wbr"])
            ods = Rot([sb(p5, "od%d" % i, [128, 3, 8, 65], F32) for i in range(2)], "od")
            obl = Rot([sb(p5, "obl%d" % i, [128, 8, 65], F32) for i in range(2)], "obl")
            oml = Rot([sb(p5, "oml%d" % i, [128, 256], F32) for i in range(2)], "oml")
            gts = Rot([sb(p5, "gt%d" % i, [128, 1280], BF16) for i in range(2)], "gt")
            rdn = Rot([sb(p5, "rdn%d" % i, [128, 16], F32) for i in range(2)], "rdn")
            oas = Rot([sb(p5, "oa%d" % i, [128, 1280], F32) for i in range(2)], "oa")
            ogs = Rot([sb(p5, "og%d" % i, [128, 1280], BF16) for i in range(2)], "og")
            ogT = Rot([sb(p5, "ogT%d" % i, [128, 10, 512], BF16) for i in range(2)], "ogT")
            sgs = Rot([sb(p5, "sg%d" % i, [128, 24, 512], BF16) for i in range(2)], "sg")
            m1s = Rot([sb(p5, "m1%d" % i, [128, 512], F32) for i in range(2)], "m1")
            m2s = Rot([sb(p5, "m2%d" % i, [128, 512], F32) for i in range(2)], "m2")
            m3s = Rot([sb(p5, "m3%d" % i, [128, 512], F32) for i in range(2)], "m3")
            mTs = Rot([sb(p5, "mT%d" % i, [128, 8, 512], BF16) for i in range(2)], "mT")
            xrs = Rot([sb(p5, "xr%d" % i, [128, D], F32) for i in range(2)], "xr")
            ys = Rot([sb(p5, "y%d" % i, [128, D], F32) for i in range(2)], "y")
            junk = sb(p5, "junk5", [128, D], BF16)
            sss = Rot([sb(p5, "ss5%d" % i, [128, 4], F32) for i in range(2)], "ss5")
            pbi = 0
            p5st = {}

            def stage_a5(tt):
                nonlocal pbi
                ogt, ogtres = ogT.next()
                sg, sgres = sgs.next()
                tr.dma("sp", sg[:], SG[:, 512 * tt:512 * tt + 512].rearrange("(c p) t -> p c t", p=128), writes=[sgres], slot=sgres)
                for s in range(4):
                    tok0 = 512 * tt + 128 * s
                    od, odres = ods.next()
                    for g in range(3):
                        tr.dma("sp", od[:, g, :, :], OD[g][tok0:tok0 + 128, :, :], writes=[odres + "g%d" % g],
                               slot="%sg%d" % (odres, g))
                    ob, obres = obl.next()
                    tr.dma("sp", ob[:], OB[tok0:tok0 + 128, :, :], writes=[obres], slot=obres)
                    oml_, omres = oml.next()
                    tr.dma("sp", oml_[:], OM[tok0:tok0 + 128, :], writes=[omres], slot=omres)
                    gt, gtres = gts.next()
                    tr.dma("sp", gt[:, 0:512], Ga[tok0:tok0 + 128, :], writes=[gtres + "a"], slot=gtres + "a")
                    tr.dma("sp", gt[:, 512:1024], Gb[tok0:tok0 + 128, :], writes=[gtres + "b"], slot=gtres + "b")
                    tr.dma("sp", gt[:, 1024:1280], Gm[tok0:tok0 + 128, :], writes=[gtres + "m"], slot=gtres + "m")
                    tr.op("dve", lambda od=od: nc.vector.tensor_tensor(out=od[:, 0], in0=od[:, 0], in1=od[:, 1], op=ALU.add),
                          reads=[odres + "g0", odres + "g1"], writes=[odres + "g0"])
                    tr.op("pool", lambda od=od: nc.gpsimd.tensor_tensor(out=od[:, 0], in0=od[:, 0], in1=od[:, 2], op=ALU.add),
                          reads=[odres + "g0", odres + "g2"], writes=[odres + "g0"])
                    rd, rdres = rdn.next()
                    tr.op("dve", lambda rd=rd, od=od: nc.vector.reciprocal(out=rd[:, 0:8], in_=od[:, 0, :, 64]),
                          reads=[odres + "g0"], writes=[rdres])
                    tr.op("dve", lambda rd=rd, ob=ob: nc.vector.reciprocal(out=rd[:, 8:16], in_=ob[:, :, 64]),
                          reads=[obres, rdres], writes=[rdres])
                    oa, oares = oas.next()
                    tr.op("dve", lambda oa=oa, od=od, rd=rd: nc.vector.tensor_tensor(
                        out=oa[:, 0:512].rearrange("p (h e) -> p h e", e=64), in0=od[:, 0, :, 0:64],
                        in1=rd[:, 0:8].unsqueeze(2).to_broadcast([128, 8, 64]), op=ALU.mult),
                        reads=[odres + "g0", rdres], writes=[oares])
                    tr.op("pool", lambda oa=oa, ob=ob, rd=rd: nc.gpsimd.tensor_tensor(
                        out=oa[:, 512:1024].rearrange("p (h e) -> p h e", e=64), in0=ob[:, :, 0:64],
                        in1=rd[:, 8:16].unsqueeze(2).to_broadcast([128, 8, 64]), op=ALU.mult),
                        reads=[obres, rdres, oares], writes=[oares])
                    og, ogres = ogs.next()
                    tr.op("dve", lambda og=og, oa=oa, gt=gt: nc.vector.tensor_tensor(
                        out=og[:, 0:1024], in0=oa[:, 0:1024], in1=gt[:, 0:1024], op=ALU.mult),
                        reads=[oares, gtres + "a", gtres + "b"], writes=[ogres])
                    tr.op("pool", lambda og=og, oml_=oml_, gt=gt: nc.gpsimd.tensor_tensor(
                        out=og[:, 1024:1280], in0=oml_[:], in1=gt[:, 1024:1280], op=ALU.mult),
                        reads=[omres, gtres + "m", ogres], writes=[ogres])
                    for fc2 in range(0, 10, 2):
                        pb = PB[pbi % 2]
                        pbres = "pb%d" % (pbi % 2)
                        pbi += 1
                        for k in range(2):
                            fc = fc2 + k
                            tr.op("pe", lambda pb=pb, og=og, fc=fc, k=k: nc.tensor.transpose(
                                out=pb[:, 128 * k:128 * k + 128], in_=og[:, 128 * fc:128 * fc + 128], identity=ident[:]),
                                reads=[ogres], writes=[pbres])
                        evac(ogt[:, fc2:fc2 + 2, 128 * s:128 * s + 128], pb[:, 0:256].rearrange("p (k t) -> p k t", t=128),
                             [pbres], [ogtres])
                p5st[tt] = (ogt, ogtres, sg, sgres)

            def stage_b5(tt):
                ogt, ogtres, sg, sgres = p5st.pop(tt)
                mT, mTres = mTs.next()
                for cc in range(8):
                    pa, pbk, pm_ = PS[0], PS[1], PS[2]
                    for (ps, pres, f0, nf) in ((pa, "ps0", 0, 4), (pbk, "ps1", 4, 4), (pm_, "ps2", 8, 2)):
                        for k in range(nf):
                            tr.op("pe", lambda ps=ps, f0=f0, k=k, nf=nf, cc=cc: nc.tensor.matmul(
                                ps[:, :], lhsT=wbr[:, f0 + k, 128 * cc:128 * cc + 128], rhs=ogt[:, f0 + k, :],
                                start=(k == 0), stop=(k == nf - 1)), reads=[ogtres, "wbr"], writes=[pres])
                    m1, m1res = m1s.next()
                    m2, m2res = m2s.next()
                    m3, m3res = m3s.next()
                    tr.op("dve", lambda m1=m1, cc=cc: nc.vector.tensor_tensor(out=m1[:], in0=pa[:, :], in1=sg[:, cc, :], op=ALU.mult),
                          reads=["ps0", sgres], writes=[m1res])
                    tr.op("dve", lambda m2=m2, cc=cc: nc.vector.tensor_tensor(out=m2[:], in0=pbk[:, :], in1=sg[:, 8 + cc, :], op=ALU.mult),
                          reads=["ps1", sgres], writes=[m2res])
                    tr.op("dve", lambda m3=m3, cc=cc: nc.vector.tensor_tensor(out=m3[:], in0=pm_[:, :], in1=sg[:, 16 + cc, :], op=ALU.mult),
                          reads=["ps2", sgres], writes=[m3res])
                    tr.op("pool", lambda m1=m1, m2=m2: nc.gpsimd.tensor_tensor(out=m1[:], in0=m1[:], in1=m2[:], op=ALU.add),
                          reads=[m1res, m2res], writes=[m1res])
                    tr.op("pool", lambda m1=m1, m3=m3, mT=mT, cc=cc: nc.gpsimd.tensor_tensor(
                        out=mT[:, cc, :], in0=m1[:], in1=m3[:], op=ALU.add), reads=[m1res, m3res], writes=[mTres])
                for s in range(4):
                    tok0 = 512 * tt + 128 * s
                    xr, xrres = xrs.next()
                    tr.dma("sp", xr[:], xs[VT - T + tok0:VT - T + tok0 + 128, :], writes=[xrres], slot=xrres)
                    y, yres = ys.next()
                    for half in range(2):
                        ps = PS[3 + half]
                        pres = "ps%d" % (3 + half)
                        for fc in range(8):
                            tr.op("pe", lambda ps=ps, fc=fc, s=s, half=half: nc.tensor.matmul(
                                ps[:, :], lhsT=mT[:, fc, 128 * s:128 * s + 128], rhs=wout[:, fc, 512 * half:512 * half + 512],
                                start=(fc == 0), stop=(fc == 7)), reads=[mTres, "wout"], writes=[pres])
                        tr.op("dve", lambda ps=ps, y=y, xr=xr, half=half: nc.vector.tensor_tensor(
                            out=y[:, 512 * half:512 * half + 512], in0=ps[:, :], in1=xr[:, 512 * half:512 * half + 512], op=ALU.add),
                            reads=[pres, xrres], writes=[yres])
                    ssb, ssres = sss.next()
                    tr.op("pool", lambda ssb=ssb: nc.gpsimd.memset(ssb[:], 0.0), writes=[ssres])
                    tr.op("act", lambda y=y, ssb=ssb: nc.scalar.activation(out=junk[:], in_=y[:], func=AF.Square, accum_out=ssb[:, 0:1]),
                          reads=[yres, ssres], writes=["junk5", ssres])
                    tr.op("act", lambda ssb=ssb: nc.scalar.activation(out=ssb[:, 1:2], in_=ssb[:, 0:1], func=AF.Sqrt, bias=EPS, scale=1.0 / D),
                          reads=[ssres], writes=[ssres])
                    tr.op("dve", lambda ssb=ssb: nc.vector.reciprocal(out=ssb[:, 2:3], in_=ssb[:, 1:2]), reads=[ssres], writes=[ssres])
                    tr.op("dve", lambda y=y, ssb=ssb: nc.vector.scalar_tensor_tensor(
                        out=y[:], in0=y[:], scalar=ssb[:, 2:3], in1=fnw[:], op0=ALU.mult, op1=ALU.mult),
                        reads=[yres, ssres, "fnw"], writes=[yres])
                    tr.dma("pool", out_d[tok0:tok0 + 128, :], y[:], reads=[yres], writes=[], slot=yres)

            stage_a5(0)
            for tt in range(8):
                if tt + 1 < 8:
                    stage_a5(tt + 1)
                stage_b5(tt)
        tr.finish()
    build_program.stats = (dict(tr.cnt), tr.nsem, tr.nwait, tr.ndma)
    return nc


def _const_tables():
    bf = ml_dtypes.bfloat16
    slopes = (2.0 ** (-8.0 * np.arange(1, 17) / 16)).astype(np.float32)
    sa = slopes[0::2].astype(np.float64)
    sbm = slopes[1::2].astype(np.float64)
    k = np.arange(128)[:, None].astype(np.float64)
    a = np.arange(128)[None, :].astype(np.float64)
    bm = np.zeros((128, 3, 8, 256), np.float32)
    for g, d in enumerate(DILS):
        for h in range(8):
            cur = np.where(k <= a, np.exp(-sa[h] * d * (a - k)), 0.0)
            prev = np.where(k >= a, np.exp(-sa[h] * d * (a + 128 - k)), 0.0)
            bm[:, g, h, 0:128] = cur
            bm[:, g, h, 128:256] = prev
    sel = (np.arange(VT)[None, :] // 256 == np.arange(64)[:, None]).astype(np.float32).astype(bf)
    p = np.arange(128)[:, None].astype(np.float64)
    m = (np.arange(NM)[None, :] - 124).astype(np.float64)
    ab = np.zeros((128, 8, NM), np.float32)
    for h in range(8):
        ab[:, h, :] = sbm[h] * (p + 128 * m)
    ct = np.zeros((128, 8, 4), np.float32)
    for h in range(8):
        for s in range(4):
            ct[:, h, s] = -MBNEG - 8.0 * sbm[h] * (p[:, 0] + 128 * s)
    pm = np.concatenate([np.zeros((128, 64), np.float32), np.full((128, 64), NEG, np.float32)], axis=1)
    cb = np.zeros((128, 4, 512), np.float32)
    tl = np.arange(512)[None, :]
    for c in range(4):
        kp = 128 * c + np.arange(128)[:, None]
        same = (tl // 256) == (c // 2)
        cb[:, c, :] = np.where(same & (kp > tl), -8e5, 0.0)
    return dict(
        ident=np.eye(128, dtype=np.float32).astype(bf), identf=np.eye(128, dtype=np.float32),
        bm=bm.reshape(128, -1), sel=sel, ab=ab.reshape(128, -1), ct=ct.reshape(128, -1), pm=pm,
        cb=cb.reshape(128, -1))


def make_in_maps(x, mem, norm_w, mem_norm_w, w_in, b_merge, w_mem_kv, w_branch_a, w_branch_b, w_branch_m, w_out,
                 final_norm_w):
    f = lambda a: np.ascontiguousarray(np.asarray(a, dtype=np.float32))
    consts = _const_tables()
    shared = dict(
        w_in=f(w_in[0]), w_mem_kv=f(w_mem_kv[0]), w_branch_a=f(w_branch_a[0]), w_branch_b=f(w_branch_b[0]),
        w_branch_m=f(w_branch_m[0]), w_out=f(w_out[0]),
        nw=f(np.asarray(norm_w[0]).reshape(8, 128).T), mnw=f(np.asarray(mem_norm_w[0]).reshape(8, 128).T),
        bmg=f(np.asarray(b_merge[0]).reshape(24, 128).T),
        fnw=f(np.broadcast_to(np.asarray(final_norm_w)[None, :], (128, D))), **consts)
    x = np.asarray(x)
    mem = np.asarray(mem)
    in_maps = []
    for c in range(8):
        b, j = c // 4, c % 4
        xsv = np.zeros((VT, D), np.float32)
        n = (j + 1) * T
        xsv[VT - n:] = x[b, :n]
        vbv = np.where(np.arange(64) >= (3 - j) * 16, 0.0, NEG).astype(np.float32)
        m = dict(shared)
        m.update(xs=xsv, memb=f(mem[b]), vb=f(np.broadcast_to(vbv[None, :], (128, 64))),
                 hv=np.full((128, 1), 1.0 if j > 0 else 0.0, np.float32))
        in_maps.append(m)
    return in_maps


def kernel(**inputs):
    in_maps = make_in_maps(**inputs)
    nc = build_program()
    res = run_bass_kernel_spmd(nc, in_maps, core_ids=list(range(8)))
    out = np.zeros((2, 4 * T, D), np.float32)
    for c in range(8):
        b, j = c // 4, c % 4
        out[b, j * T:(j + 1) * T] = res.results[c]["out"]
    return out
```
